# Optimizing a Trainium2 kernel written in Bass

```python
import jax, jax.numpy as jnp
from jax import lax
import numpy as np

D_MODEL = 2048
BATCH = 2
SEQ = 4096
DEPTH = 1

HEAD_DIM = 128
EPS = 1e-6
A_PATTERNS = ((128, 1), (512, 4), (2048, 16))
A_N_GROUPS = len(A_PATTERNS)
A_HEADS = 4
A_WIDTH = A_HEADS * HEAD_DIM
ROPE_THETA = 500000.0
ROPE_DIMS = HEAD_DIM // 4
B_Q_HEADS = 16
B_KV_HEADS = 4
B_GROUP = B_Q_HEADS // B_KV_HEADS
B_Q_WIDTH = B_Q_HEADS * HEAD_DIM
B_KV_WIDTH = B_KV_HEADS * HEAD_DIM
AXIAL_THETA = 10000.0
AXIAL_DIMS = HEAD_DIM // 2
GRID_W = 64
Q_BLOCK = 128
A_COLS = A_N_GROUPS * 3 * A_WIDTH
B_COLS = B_Q_WIDTH + 2 * B_KV_WIDTH
GATE_COLS = 2 * D_MODEL
IN_COLS = A_COLS + B_COLS + GATE_COLS
N_MOD = 6
N_EXPERTS = 64
N_EXPERT_GROUPS = 8
TOPK_GROUPS = 4
TOP_K = 8
D_EXPERT = D_MODEL // 4
D_SHARED = D_MODEL // 4
ROUTED_SCALE = 2.5
MOE_BLOCK = 128
NEG = -1e30

kernel_name = 'hybrid_dilated_gqa_moe_block'


def rmsnorm(x, g):
    xf = x.astype(jnp.float32)
    y = xf * lax.rsqrt(jnp.mean(xf * xf, axis=-1, keepdims=True) + EPS)
    return (y * g.astype(jnp.float32)).astype(x.dtype)


def rotate(x, cos, sin):
    half = x.shape[-1] // 2
    x1, x2 = x[..., :half], x[..., half:]
    return jnp.concatenate([x1 * cos - x2 * sin, x2 * cos + x1 * sin], axis=-1)


def rope_angles(pos, dims, theta):
    inv = jnp.power(jnp.float32(theta), -jnp.arange(0, dims, 2, dtype=jnp.float32) / dims)
    return pos.astype(jnp.float32)[:, None] * inv[None, :]


def partial_rope(x):
    s = x.shape[2]
    ang = rope_angles(jnp.arange(s), ROPE_DIMS, ROPE_THETA)
    cos, sin = jnp.cos(ang).astype(x.dtype), jnp.sin(ang).astype(x.dtype)
    return jnp.concatenate([rotate(x[..., :ROPE_DIMS], cos, sin), x[..., ROPE_DIMS:]], axis=-1)


def axial_rope(x):
    s = x.shape[1]
    rows = s // GRID_W
    row_id = jnp.broadcast_to(jnp.arange(rows)[:, None], (rows, GRID_W)).reshape(-1)
    col_id = jnp.broadcast_to(jnp.arange(GRID_W)[None, :], (rows, GRID_W)).reshape(-1)
    ang_r = rope_angles(row_id, AXIAL_DIMS, AXIAL_THETA)[:, None, :]
    ang_c = rope_angles(col_id, AXIAL_DIMS, AXIAL_THETA)[:, None, :]
    xr = rotate(x[..., :AXIAL_DIMS], jnp.cos(ang_r).astype(x.dtype), jnp.sin(ang_r).astype(x.dtype))
    xc = rotate(x[..., AXIAL_DIMS:], jnp.cos(ang_c).astype(x.dtype), jnp.sin(ang_c).astype(x.dtype))
    return jnp.concatenate([xr, xc], axis=-1)


def banded_attention(q, k, v, half_window):
    b, h, L, hd = q.shape
    P = half_window
    nb = -(-L // P)
    Lp = nb * P
    qb = jnp.pad(q, ((0, 0), (0, 0), (0, Lp - L), (0, 0))).reshape(b, h, nb, P, hd)
    kv_pad = ((0, 0), (0, 0), (P, Lp - L + P), (0, 0))
    kb = jnp.pad(k, kv_pad).reshape(b, h, nb + 2, P, hd)
    vb = jnp.pad(v, kv_pad).reshape(b, h, nb + 2, P, hd)
    kwin = jnp.concatenate([kb[:, :, :-2], kb[:, :, 1:-1], kb[:, :, 2:]], axis=3)
    vwin = jnp.concatenate([vb[:, :, :-2], vb[:, :, 1:-1], vb[:, :, 2:]], axis=3)
    q_pos = jnp.arange(Lp).reshape(nb, P)
    k_pos = jnp.arange(nb)[:, None] * P - P + jnp.arange(3 * P)[None, :]
    kp = k_pos[:, None, :]
    valid = (jnp.abs(q_pos[:, :, None] - kp) <= half_window) & (kp >= 0) & (kp < L)
    sc = jnp.einsum('bhnqd,bhnkd->bhnqk', qb, kwin).astype(jnp.float32) * (hd ** -0.5)
    sc = jnp.where(valid, sc, NEG)
    lse = jax.nn.logsumexp(sc, axis=-1)
    p = jnp.exp(sc - lse[..., None])
    o = jnp.einsum('bhnqk,bhnkd->bhnqd', p.astype(v.dtype), vwin)
    return o.reshape(b, h, Lp, hd)[:, :, :L], lse.reshape(b, h, Lp)[:, :, :L]


def dilated_attention(q, k, v, window, dilation):
    b, h, s, hd = q.shape
    L = s // dilation

    def split(z):
        return z.reshape(b, h, L, dilation, hd).transpose(0, 1, 3, 2, 4).reshape(b, h * dilation, L, hd)

    o, lse = banded_attention(split(q), split(k), split(v), (window // 2) // dilation)
    o = o.reshape(b, h, dilation, L, hd).transpose(0, 1, 3, 2, 4).reshape(b, h, s, hd)
    lse = lse.reshape(b, h, dilation, L).transpose(0, 1, 3, 2).reshape(b, h, s)
    return o, lse


def mixer_a(a_cols):
    b, s, _ = a_cols.shape
    a = a_cols.reshape(b, s, A_N_GROUPS, 3, A_HEADS, HEAD_DIM).transpose(2, 3, 0, 4, 1, 5)
    outs, lses = [], []
    for gi, (window, dilation) in enumerate(A_PATTERNS):
        q = partial_rope(a[gi, 0])
        k = partial_rope(a[gi, 1])
        o, lse = dilated_attention(q, k, a[gi, 2], window, dilation)
        outs.append(o)
        lses.append(lse)
    w = jax.nn.softmax(jnp.stack(lses, axis=0), axis=0)
    o = jnp.sum(w[..., None].astype(outs[0].dtype) * jnp.stack(outs, axis=0), axis=0)
    return o.transpose(0, 2, 1, 3).reshape(b, s, A_WIDTH)


def mixer_b(qc, kc, vc, q_norm_g, k_norm_g):
    b, s, _ = qc.shape
    q = axial_rope(rmsnorm(qc.reshape(b, s, B_Q_HEADS, HEAD_DIM), q_norm_g))
    k = axial_rope(rmsnorm(kc.reshape(b, s, B_KV_HEADS, HEAD_DIM), k_norm_g))
    v = vc.reshape(b, s, B_KV_HEADS, HEAD_DIM).transpose(0, 2, 1, 3)
    k = k.transpose(0, 2, 1, 3)
    q = q.reshape(b, s, B_KV_HEADS, B_GROUP, HEAD_DIM).transpose(0, 2, 3, 1, 4)
    nblk = s // Q_BLOCK
    qb = q.reshape(b, B_KV_HEADS, B_GROUP, nblk, Q_BLOCK, HEAD_DIM).transpose(3, 0, 1, 2, 4, 5)
    scale = HEAD_DIM ** -0.5

    def one_block(qblk):
        sc = jnp.einsum('bkgqd,bksd->bkgqs', qblk, k).astype(jnp.float32) * scale
        p = jax.nn.softmax(sc, axis=-1)
        return jnp.einsum('bkgqs,bksd->bkgqd', p.astype(v.dtype), v)

    o = lax.map(one_block, qb)
    return o.transpose(1, 0, 4, 2, 3, 5).reshape(b, s, B_Q_WIDTH)


def moe(h, w_router, e_bias, w1, w3, w2, ws1, ws3, ws2):
    b, s, d = h.shape
    t = h.reshape(b * s, d)
    n_tok = t.shape[0]
    scores = jax.nn.sigmoid((t @ w_router).astype(jnp.float32))
    sel = scores + e_bias.astype(jnp.float32)
    per_group = N_EXPERTS // N_EXPERT_GROUPS
    grp_score = jnp.sum(lax.top_k(sel.reshape(n_tok, N_EXPERT_GROUPS, per_group), 2)[0], axis=-1)
    _, top_g = lax.top_k(grp_score, TOPK_GROUPS)
    gmask = jnp.any(top_g[:, :, None] == jnp.arange(N_EXPERT_GROUPS)[None, None, :], axis=1)
    emask = jnp.repeat(gmask, per_group, axis=1)
    _, top_e = lax.top_k(jnp.where(emask, sel, -jnp.inf), TOP_K)
    chosen = jnp.take_along_axis(scores, top_e, axis=1)
    wts = chosen / jnp.sum(chosen, axis=-1, keepdims=True) * ROUTED_SCALE
    gate = jnp.sum(jax.nn.one_hot(top_e, N_EXPERTS, dtype=jnp.float32) * wts[..., None], axis=1)
    gate = gate.astype(t.dtype)
    nb = n_tok // MOE_BLOCK

    def expert_block(args):
        tb, gb = args
        a = jnp.einsum('td,edf->tef', tb, w1)
        u = jnp.einsum('td,edf->tef', tb, w3)
        hid = jax.nn.silu(a) * u * gb[..., None]
        return jnp.einsum('tef,efd->td', hid, w2)

    routed = lax.map(expert_block, (t.reshape(nb, MOE_BLOCK, d), gate.reshape(nb, MOE_BLOCK, N_EXPERTS)))
    shared = (jax.nn.silu(t @ ws1) * (t @ ws3)) @ ws2
    return (routed.reshape(n_tok, d) + shared).reshape(b, s, d)


def setup_inputs(seed: int = 0) -> dict:
    key = jax.random.key(seed)
    ks = jax.random.split(key, 24)
    f32 = jnp.float32

    def nrm(k, shape, fan_in, mult=1.0):
        return jax.random.normal(k, shape, f32) * (mult * fan_in ** -0.5)

    L = DEPTH
    return {
        'x': jax.random.normal(ks[0], (BATCH, SEQ, D_MODEL), f32),
        'c': jax.random.normal(ks[1], (BATCH, D_MODEL), f32),
        'w_ada': nrm(ks[2], (L, D_MODEL, N_MOD * D_MODEL), D_MODEL, 0.5),
        'b_ada': 0.02 * jax.random.normal(ks[3], (L, N_MOD * D_MODEL), f32),
        'g_attn': 1.0 + 0.05 * jax.random.normal(ks[4], (L, D_MODEL), f32),
        'w_in': nrm(ks[5], (L, D_MODEL, IN_COLS), D_MODEL),
        'b_gate': 0.02 * jax.random.normal(ks[6], (L, GATE_COLS), f32),
        'q_norm_g': 1.0 + 0.05 * jax.random.normal(ks[7], (L, HEAD_DIM), f32),
        'k_norm_g': 1.0 + 0.05 * jax.random.normal(ks[8], (L, HEAD_DIM), f32),
        'w_a_up': nrm(ks[9], (L, A_WIDTH, D_MODEL), A_WIDTH),
        'w_b_up': nrm(ks[10], (L, B_Q_WIDTH, D_MODEL), B_Q_WIDTH),
        'w_out': nrm(ks[11], (L, D_MODEL, D_MODEL), D_MODEL),
        'g_ffn': 1.0 + 0.05 * jax.random.normal(ks[12], (L, D_MODEL), f32),
        'w_router': nrm(ks[13], (L, D_MODEL, N_EXPERTS), D_MODEL),
        'e_bias': 0.01 * jax.random.normal(ks[14], (L, N_EXPERTS), f32),
        'w1': nrm(ks[15], (L, N_EXPERTS, D_MODEL, D_EXPERT), D_MODEL),
        'w3': nrm(ks[16], (L, N_EXPERTS, D_MODEL, D_EXPERT), D_MODEL),
        'w2': nrm(ks[17], (L, N_EXPERTS, D_EXPERT, D_MODEL), D_EXPERT),
        'ws1': nrm(ks[18], (L, D_MODEL, D_SHARED), D_MODEL),
        'ws3': nrm(ks[19], (L, D_MODEL, D_SHARED), D_MODEL),
        'ws2': nrm(ks[20], (L, D_SHARED, D_MODEL), D_SHARED),
        'g_final': 1.0 + 0.05 * jax.random.normal(ks[21], (D_MODEL,), f32),
    }


def reference(x, c, w_ada, b_ada, g_attn, w_in, b_gate, q_norm_g, k_norm_g, w_a_up, w_b_up, w_out,
              g_ffn, w_router, e_bias, w1, w3, w2, ws1, ws3, ws2, g_final):
    c_act = jax.nn.silu(c)
    for l in range(DEPTH):
        mod = c_act @ w_ada[l] + b_ada[l]
        sh1, sc1, gt1, sh2, sc2, gt2 = jnp.split(mod[:, None, :], N_MOD, axis=-1)
        h = rmsnorm(x, g_attn[l]) * (1.0 + sc1) + sh1
        proj = h @ w_in[l]
        o1 = A_COLS
        o2 = o1 + B_Q_WIDTH
        o3 = o2 + B_KV_WIDTH
        o4 = o3 + B_KV_WIDTH
        y_a = mixer_a(proj[..., :o1]) @ w_a_up[l]
        y_b = mixer_b(proj[..., o1:o2], proj[..., o2:o3], proj[..., o3:o4], q_norm_g[l], k_norm_g[l]) @ w_b_up[l]
        gates = jax.nn.sigmoid(proj[..., o4:] + b_gate[l])
        merged = gates[..., :D_MODEL] * y_a + gates[..., D_MODEL:] * y_b
        x = x + gt1 * (merged @ w_out[l])
        h2 = rmsnorm(x, g_ffn[l]) * (1.0 + sc2) + sh2
        x = x + gt2 * moe(h2, w_router[l], e_bias[l], w1[l], w3[l], w2[l], ws1[l], ws3[l], ws2[l])
    return rmsnorm(x, g_final)
```

```python
import contextlib
import numpy as np
import concourse.bass as bass
import concourse.mybir as mybir
from concourse.bass_utils import run_bass_kernel_spmd

F32, BF16 = mybir.dt.float32, mybir.dt.bfloat16
AF = mybir.ActivationFunctionType
ALU = mybir.AluOpType
AX = mybir.AxisListType

D = 2048
S = 4096
NT = 1024
HD = 128
EPS = 1e-6
NE = 64
IN_COLS = 11776
A_DIL = (1, 4, 16)
ARENA = 103200
SCALE = float(HD) ** -0.5


class T:
    __slots__ = ("w", "r", "name")

    def __init__(self, name=""):
        self.w = None
        self.r = {}
        self.name = name


def alias(new_tiles, old_tiles):
    evs = {}
    for o in old_tiles:
        if o.w is not None:
            k, v = o.w
            evs[k] = max(evs.get(k, 0), v)
        for k, v in o.r.items():
            evs[k] = max(evs.get(k, 0), v)
    for n in new_tiles:
        for k, v in evs.items():
            if n.r.get(k, 0) < v:
                n.r[k] = v


class StopBuild(Exception):
    pass


class Prog:
    ENG = ("pe", "act", "dve", "pool", "sp")

    def __init__(self):
        self.q = {e: [] for e in self.ENG}
        self.cnt = {}
        self.waited = {e: {} for e in self.ENG}
        self.dpool_i = {}

    def _waits(self, eng, reads, writes):
        waits = {}

        def need(k, v):
            if waits.get(k, 0) < v:
                waits[k] = v
        for t in reads:
            if t.w is not None:
                need(*t.w)
        for t in writes:
            if t.w is not None:
                need(*t.w)
            for k, v in t.r.items():
                need(k, v)
        wl = []
        for k, v in waits.items():
            if k == eng and eng == "pe":
                continue
            if self.waited[eng].get(k, 0) >= v:
                continue
            self.waited[eng][k] = v
            wl.append((k, v))
        return wl

    def _commit(self, ev, reads, writes):
        k, v = ev
        for t in reads:
            if t.r.get(k, 0) < v:
                t.r[k] = v
        for t in writes:
            t.w = ev
            t.r = {}

    def op(self, eng, fn, reads=(), writes=()):
        reads = list(reads)
        writes = list(writes)
        wl = self._waits(eng, reads, writes)
        self.cnt[eng] = self.cnt.get(eng, 0) + 1
        ev = (eng, self.cnt[eng])
        self.q[eng].append((wl, fn, (eng, 1)))
        self._commit(ev, reads, writes)
        return ev

    NDSEM = {"sp": 24, "pool": 24}

    def dma(self, eng, sem, out, in_, reads=(), writes=()):
        reads = list(reads)
        writes = list(writes)
        i = self.dpool_i.get(eng, 0)
        self.dpool_i[eng] = i + 1
        sem = "%s_d%d" % (eng, i % self.NDSEM[eng])
        wl = self._waits(eng, reads, writes)
        prev = self.cnt.get(sem, 0)
        if prev > 0 and self.waited[eng].get(sem, 0) < prev:
            self.waited[eng][sem] = prev
            wl.append((sem, prev))
        self.cnt[sem] = prev + 16
        ev = (sem, self.cnt[sem])
        self.q[eng].append((wl, (lambda e, o=out, i=in_: e.dma_start(out=o, in_=i)), (sem, 16)))
        self._commit(ev, reads, writes)
        return ev

    def final_wait(self, eng, sem):
        self.q[eng].append(([(sem, self.cnt[sem])], None, None))

    def replay(self, name, e, sems):
        for wl, fn, inc in self.q[name]:
            for k, v in wl:
                e.wait_ge(sems[k], v)
            if fn is None:
                continue
            ins = fn(e)
            ins.then_inc(sems[inc[0]], inc[1])


def build_nc(stop_after=None, dbg=None):
    nc = bass.Bass("TRN2", target_bir_lowering=False)
    P = Prog()

    def din(name, shape):
        return nc.dram_tensor(name, list(shape), F32, kind="ExternalInput").ap()

    xr = din("xr", [S, D])
    c_pl = din("c_pl", [128, 16])
    w_ada = din("w_ada", [D, 6 * D])
    bada_pl_d = din("bada_pl", [128, 96])
    gattn_d = din("gattn_pl", [128, 16])
    gffn_d = din("gffn_pl", [128, 16])
    w_in = din("w_in", [D, IN_COLS])
    bgate_d = din("bgate_pl", [128, 32])
    qg_d = din("qg", [1, HD])
    kg_d = din("kg", [1, HD])
    w_a_up = din("w_a_up", [512, D])
    w_b_up = din("w_b_up", [D, D])
    w_out = din("w_out", [D, D])
    w_router = din("w_router", [D, NE])
    ebias_d = din("e_bias", [1, NE])
    w1 = din("w1", [NE, D, 512])
    w3 = din("w3", [NE, D, 512])
    w2 = din("w2", [NE, 512, D])
    ws1 = din("ws1", [D, 512])
    ws3 = din("ws3", [D, 512])
    ws2 = din("ws2", [512, D])
    gfin_d = din("g_final", [1, D])
    ident_d = din("ident", [128, 128])
    mask2_d = din("mask2", [128, 256])
    tabA_d = din("tabA", [S, 64])
    tabB_d = din("tabB", [S, 256])
    kv0_d = din("kv0", [128, 9])
    kv1_d = din("kv1", [128, 12])
    kv2_d = din("kv2", [128, 32])
    out_d = nc.dram_tensor("out", [NT, D], F32, kind="ExternalOutput").ap()

    es = contextlib.ExitStack()
    arena = es.enter_context(nc.sbuf_tensor("arena", [128, ARENA], BF16))
    psum = es.enter_context(nc.psum_tensor("psum", [128, 8, 512], F32))

    def V(off, shape, dt=BF16):
        n = 1
        for s_ in shape[1:]:
            n *= s_
        assert off % 4 == 0
        assert off + n * (4 if dt == F32 else 2) <= ARENA * 2, (off, shape)
        if dt == F32:
            a = arena[:, off // 2: off // 2 + 2 * n].bitcast(F32)
        else:
            a = arena[:, off // 2: off // 2 + n]
        if len(shape) == 3:
            a = a.rearrange("p (a b) -> p a b", b=shape[2])
        elif len(shape) == 4:
            a = a.rearrange("p (a b c) -> p a b c", b=shape[2], c=shape[3])
        if shape[0] < 128:
            a = a[0:shape[0]]
        return a

    def emit(dump=None):
        if dump is not None:
            ap, tiles = dump
            ncol = ap.shape[1]
            P.dma("sp", "do", out_d[0:ap.shape[0], 0:ncol], ap, reads=tiles)
        for sn in list(P.cnt.keys()):
            P.final_wait("sp", sn)
        sem_names = ["pe", "act", "dve"] + ["sp_d%d" % i for i in range(24)] + ["pool_d%d" % i for i in range(24)]
        sems = {n_: es.enter_context(nc.semaphore("s_" + n_)) for n_ in sem_names}
        with nc.Block() as block:
            @block.tensor
            def _(e):
                P.replay("pe", e, sems)

            @block.scalar
            def _(e):
                P.replay("act", e, sems)

            @block.vector
            def _(e):
                P.replay("dve", e, sems)

            @block.gpsimd
            def _(e):
                P.replay("pool", e, sems)

            @block.sync
            def _(e):
                P.replay("sp", e, sems)
        es.close()
        nc._prog_counts = dict(P.cnt)
        nc._prog = P
        return nc

    PB = [T("pb%d" % i) for i in range(8)]

    def pbank(b, dt=F32):
        a = psum[:, b, :]
        return a.bitcast(BF16) if dt == BF16 else a

    def pbanks(b0, nb):
        return psum[:, b0:b0 + nb, :].rearrange("p a b -> p (a b)")

    CB = 5632
    RING_ADDR = [CB, CB + 16384, CB + 32768, 120320]
    XT = 54784
    XN = 71168
    HTT = 79360
    HTO = 87552
    OATA = 120320
    TMP = 128512
    KBF = 136704
    TAB = 138752
    BIG = 140800

    ident_bf = V(0, [128, 128]); ones_bf = V(256, [128, 128])
    ident_f = V(512, [128, 128], F32); mask2 = V(1024, [128, 256], F32)
    modT = V(2048, [128, 96], F32); A1 = V(2432, [128, 16], F32); A2 = V(2496, [128, 16], F32)
    c_act = V(2560, [128, 16], F32); gattn = V(2624, [128, 16], F32); gffn = V(2688, [128, 16], F32)
    bada_pl = V(2752, [128, 96], F32); bgate = V(3136, [128, 32], F32)
    qg_b = V(3264, [128, 128], F32); kg_b = V(3776, [128, 128], F32); ebias_b = V(4288, [128, 64], F32)
    kv0 = V(4544, [128, 9], F32); kv1 = V(4608, [128, 12], F32); kv2 = V(4672, [128, 32], F32)
    ones_f = V(4800, [128, 128], F32)
    scr = V(5312, [128, 64], F32)
    t_const = T("const"); t_modT = T("modT"); t_A1 = T("A1"); t_A2 = T("A2"); t_cact = T("cact")

    P.dma("sp", "dc", c_act, c_pl, writes=[t_cact])
    for dst, src in ((ident_f, ident_d), (mask2, mask2_d), (gattn, gattn_d), (gffn, gffn_d),
                     (bada_pl, bada_pl_d), (bgate, bgate_d), (kv0, kv0_d), (kv1, kv1_d), (kv2, kv2_d),
                     (qg_b, qg_d[0].partition_broadcast(128)), (kg_b, kg_d[0].partition_broadcast(128)),
                     (ebias_b, ebias_d[0].partition_broadcast(128))):
        P.dma("sp", "dc", dst, src, writes=[t_const])
    P.dma("pool", "dw", ident_bf, ident_d, writes=[t_const])
    P.op("dve", lambda e: e.memset(ones_bf, 1.0), writes=[t_const])
    P.op("dve", lambda e: e.memset(ones_f, 1.0), writes=[t_const])
    P.op("act", lambda e: e.activation(out=c_act, in_=c_act, func=AF.Silu), reads=[t_cact], writes=[t_cact])

    if stop_after == 'c0':
        return emit((c_act, [t_cact, t_const]))
    scr_t = [T("scr%d" % i) for i in range(16)]
    scr_i = [0]

    def scratch():
        i = scr_i[0] % 16
        scr_i[0] += 1
        return scr[:, i * 4:i * 4 + 4], scr_t[i]

    ring_t = [T("ring%d" % i) for i in range(4)]
    ring_i = [0]
    ring_n = [3]

    def wslot():
        i = ring_i[0] % ring_n[0]
        ring_i[0] += 1
        return RING_ADDR[i], ring_t[i]

    def wload(src_ap, shape):
        off, t = wslot()
        v = V(off, shape)
        P.dma("pool", "dw", v, src_ap, writes=[t])
        return v, t

    def w_cols(w, c0, n):
        return w[:, c0:c0 + n].rearrange("(k p) n -> p k n", p=128)

    ACC = BIG
    STG = BIG + 24576
    acc = V(ACC, [128, 6144], F32)
    t_acc = T("acc")
    stg_t = [T("stg0"), T("stg1")]
    stg_i = [0]

    def mod_chunk(half, k, pc):
        i = stg_i[0] % 2
        stg_i[0] += 1
        st = V(STG + i * 6144, [128, 1536], F32)
        col0 = half * 6144 + pc * 1536
        P.dma("sp", "dx", st, w_ada[k * 128:(k + 1) * 128, col0:col0 + 1536], writes=[stg_t[i]])
        a = acc[:, pc * 1536:(pc + 1) * 1536]
        if k == 0:
            P.op("dve", lambda e, a=a, st=st: e.tensor_scalar(out=a, in0=st, scalar1=c_act[:, 0:1], scalar2=None,
                                                                op0=ALU.mult),
                 reads=[stg_t[i], t_cact], writes=[t_acc])
        else:
            P.op("dve", lambda e, a=a, st=st, k=k: e.scalar_tensor_tensor(out=a, in0=st, scalar=c_act[:, k:k + 1],
                                                                          in1=a, op0=ALU.mult, op1=ALU.add),
                 reads=[stg_t[i], t_cact], writes=[t_acc])

    def mod_finish(half):
        pm = pbank(7)

        def f(e):
            ins = None
            for j in range(48):
                ins = e.matmul(pm[:, j:j + 1], lhsT=acc[:, j * 128:(j + 1) * 128], rhs=ones_f[:, 0:1],
                               start=True, stop=True)
            return ins
        P.op("pe", f, reads=[t_acc, t_const], writes=[PB[7]])
        P.op("dve", lambda e: e.tensor_tensor(out=modT[:, half * 48:half * 48 + 48], in0=pm[:, 0:48],
                                              in1=bada_pl[:, half * 48:half * 48 + 48], op=ALU.add),
             reads=[PB[7], t_const], writes=[t_modT])

    for k in range(16):
        for pc in range(4):
            mod_chunk(0, k, pc)
    if stop_after == 'c1':
        return emit((acc[:, 0:2048], [t_acc]))
    mod_finish(0)
    if stop_after == 'c2':
        return emit((modT, [t_modT]))
    P.op("dve", lambda e: e.scalar_tensor_tensor(out=A1, in0=modT[:, 16:32], scalar=1.0, in1=gattn,
                                                 op0=ALU.add, op1=ALU.mult),
         reads=[t_modT, t_const], writes=[t_A1])
    B1 = modT[:, 0:16]
    B2 = modT[:, 48:64]
    if stop_after == 'mod1':
        return emit((modT, [t_modT]))
    if stop_after == 'mod1b':
        return emit((A1, [t_A1]))

    h2_chunks = [(k, pc) for k in range(16) for pc in range(4)]
    h2_state = {"i": 0, "done": False}

    def mod_h2_step(n):
        for _ in range(n):
            if h2_state["i"] < len(h2_chunks):
                k, pc = h2_chunks[h2_state["i"]]
                mod_chunk(1, k, pc)
                h2_state["i"] += 1
        if h2_state["i"] == len(h2_chunks) and not h2_state["done"]:
            h2_state["done"] = True
            mod_finish(1)
            P.op("dve", lambda e: e.scalar_tensor_tensor(out=A2, in0=modT[:, 64:80], scalar=1.0, in1=gffn,
                                                         op0=ALU.add, op1=ALU.mult),
                 reads=[t_modT, t_const], writes=[t_A2])

    xt_t = [T("xt0"), T("xt1")]; xn_t = [T("xn0"), T("xn1")]; htt_t = [T("htt0"), T("htt1")]
    prod_i = [0]

    def rms_rstd(src_ap, n, width, src_tiles, junk_ap, junk_tiles):
        ss, tss = scratch()
        P.op("act", lambda e: e.activation(out=junk_ap, in_=src_ap, func=AF.Square, accum_out=ss[0:n, 0:1]),
             reads=src_tiles, writes=junk_tiles + [tss])
        P.op("act", lambda e: e.activation(out=ss[0:n, 1:2], in_=ss[0:n, 0:1], func=AF.Sqrt, scale=1.0 / width,
                                           bias=EPS), reads=[tss], writes=[tss])
        P.op("dve", lambda e: e.reciprocal(out=ss[0:n, 2:3], in_=ss[0:n, 1:2]), reads=[tss], writes=[tss])
        return ss[0:n, 2:3], tss

    htmp_addr = [BIG + 61440]
    t_htmp = T("htmp")

    def transpose_mod(xn, n, xn_tiles, dst, dst_t, Asc, Bsc, t_sc, pb):
        tp = pbanks(pb, 2).bitcast(BF16).rearrange("p (k n) -> p k n", n=128)
        htmp = V(htmp_addr[0], [128, 16, 128])

        def f(e):
            ins = None
            for k in range(16):
                ins = e.transpose(out=tp[:, k, 0:n], in_=xn[:, k * 128:(k + 1) * 128], identity=ident_bf[0:n, 0:n])
            return ins
        P.op("pe", f, reads=xn_tiles + [t_const], writes=[PB[pb], PB[pb + 1]])
        P.op("act", lambda e: e.activation(out=htmp[:, :, 0:n], in_=tp[:, :, 0:n], func=AF.Identity),
             reads=[PB[pb], PB[pb + 1]], writes=[t_htmp])
        P.op("dve", lambda e: e.tensor_tensor(out=htmp[:, :, 0:n], in0=htmp[:, :, 0:n],
                                              in1=Asc.unsqueeze(2).to_broadcast([128, 16, n]), op=ALU.mult),
             reads=[t_sc, t_modT], writes=[t_htmp])
        P.op("dve", lambda e: e.tensor_tensor(out=dst[:, :, 0:n], in0=htmp[:, :, 0:n],
                                              in1=Bsc.unsqueeze(2).to_broadcast([128, 16, n]), op=ALU.add),
             reads=[t_htmp, t_sc, t_modT], writes=[dst_t])

    def produce_a1(rows_ap, n):
        i = prod_i[0] % 2
        prod_i[0] += 1
        xt = V(XT + i * 8192, [128, 2048], F32)[0:n]
        xn = V(XN + i * 4096, [128, 2048])[0:n]
        P.dma("sp", "dx", xt, rows_ap, writes=[xt_t[i]])
        rstd, trs = rms_rstd(xt, n, D, [xt_t[i]], xn, [xn_t[i]])
        P.op("act", lambda e: e.activation(out=xn, in_=xt, func=AF.Identity, scale=rstd),
             reads=[xt_t[i], trs], writes=[xn_t[i]])
        return (xn, n, i)

    def produce_a2(a1, dst, dst_t):
        xn, n, i = a1
        transpose_mod(xn, n, [xn_t[i]], dst, dst_t, A1, B1, t_A1, i * 2)

    def produce(rows_ap, n, dst, dst_t):
        produce_a2(produce_a1(rows_ap, n), dst, dst_t)

    def produce_htt_a2(a1):
        si = a1[2]
        htt = V(HTT + si * 4096, [128, 16, 128])
        produce_a2(a1, htt, htt_t[si])
        return htt, htt_t[si]

    hT_own = V(HTO, [128, 16, 1024])
    hto_t = [T("hto%d" % i) for i in range(8)]
    for j in range(8):
        produce(xr[1024 + j * 128:1024 + (j + 1) * 128, :], 128, hT_own[:, :, j * 128:(j + 1) * 128], hto_t[j])
        mod_h2_step(8)

    if stop_after == 'p1a':
        dd = V(TMP, [128, 1024], F32)
        t_dd = T('dd')
        P.op('dve', lambda e: e.tensor_copy(out=dd.rearrange('p (a b) -> p a b', b=128), in_=hT_own[:, 0:8, 0:128]), reads=hto_t, writes=[t_dd])
        P.op('dve', lambda e: e.tensor_copy(out=dd[:, 0:96], in_=modT), reads=[t_modT], writes=[t_dd])
        return emit((dd, [t_dd]))
    def proj_tm(hT_ap, n, h_tiles, wv, wt, pb):
        po = pbank(pb)

        def f(e):
            ins = None
            for k in range(16):
                ins = e.matmul(po[0:n, :], lhsT=hT_ap[:, k, 0:n], rhs=wv[:, k, :], start=(k == 0), stop=(k == 15))
            return ins
        P.op("pe", f, reads=list(h_tiles) + [wt], writes=[PB[pb]])
        return po

    tmpv = [V(TMP + i * 2048, [128, 512], F32) for i in range(4)]
    tmp_t = [T("tmp%d" % i) for i in range(4)]
    kbf = [V(KBF + i * 1024, [128, 512]) for i in range(2)]
    kbf_t = [T("kbf0"), T("kbf1")]
    kbf_i = [0]
    tab_t = [T("tab0"), T("tab1")]
    tab_i = [0]

    def load_tab(src_ap, n, width):
        i = tab_i[0] % 2
        tab_i[0] += 1
        tv = V(TAB + i * 1024, [128, 256], F32)[0:n, 0:width]
        P.dma("sp", "dx", tv, src_ap, writes=[tab_t[i]])
        return tv, tab_t[i]

    def transpose_heads(kb, n, kt, evac_to_T, pb=6):
        tp = pbank(pb, BF16)[:, 0:512].rearrange("p (h n) -> p h n", n=128)

        def f(e):
            ins = None
            for h in range(4):
                ins = e.transpose(out=tp[:, h, 0:n], in_=kb[:, h * 128:(h + 1) * 128], identity=ident_bf[0:n, 0:n])
            return ins
        P.op("pe", f, reads=[kt, t_const], writes=[PB[pb]])
        evac_to_T(tp, PB[pb])

    def rope_A(po, n, pb, tab, tt, evac_to_T=None):
        i = kbf_i[0] % 2
        kbf_i[0] += 1
        kb = kbf[i][0:n]
        kb3 = kb.rearrange("p (h d) -> p h d", d=128)
        po3 = po[0:n, :].rearrange("p (h d) -> p h d", d=128)
        kf = tmpv[2][0:n, :]
        kf3 = kf.rearrange("p (h d) -> p h d", d=128)
        P.op("act", lambda e: e.activation(out=kf, in_=po[0:n, :], func=AF.Identity), reads=[PB[pb]],
             writes=[tmp_t[2]])
        P.op("dve", lambda e: e.tensor_copy(out=kb, in_=kf), reads=[tmp_t[2]], writes=[kbf_t[i]])
        t1 = tmpv[0][0:n, 0:128].rearrange("p (h d) -> p h d", d=32)
        t2 = tmpv[1][0:n, 0:128].rearrange("p (h d) -> p h d", d=32)
        c1 = tab[:, 0:32].unsqueeze(1).to_broadcast([n, 4, 32])
        s_lo = tab[:, 32:48].unsqueeze(1).to_broadcast([n, 4, 16])
        s_hi = tab[:, 48:64].unsqueeze(1).to_broadcast([n, 4, 16])
        P.op("dve", lambda e: e.tensor_tensor(out=t1, in0=kf3[:, :, 0:32], in1=c1, op=ALU.mult),
             reads=[tmp_t[2], tt], writes=[tmp_t[0]])
        P.op("dve", lambda e: e.tensor_tensor(out=t2[:, :, 0:16], in0=kf3[:, :, 16:32], in1=s_lo, op=ALU.mult),
             reads=[tmp_t[2], tt], writes=[tmp_t[1]])
        P.op("dve", lambda e: e.tensor_tensor(out=t2[:, :, 16:32], in0=kf3[:, :, 0:16], in1=s_hi, op=ALU.mult),
             reads=[tmp_t[2], tt], writes=[tmp_t[1]])
        P.op("dve", lambda e: e.tensor_tensor(out=kb3[:, :, 0:32], in0=t1, in1=t2, op=ALU.add),
             reads=[tmp_t[0], tmp_t[1]], writes=[kbf_t[i]])
        if evac_to_T is None:
            return kb, kbf_t[i]
        transpose_heads(kb, n, kbf_t[i], evac_to_T)

    def rope_B(po, n, pb, tab, tt, g_b, evac_to_T=None, on_dve=False):
        i = kbf_i[0] % 2
        kbf_i[0] += 1
        kb = kbf[i][0:n]
        po3 = po[0:n, :].rearrange("p (h d) -> p h d", d=128)
        ss, tss = scratch()
        junk = tmpv[0][0:n, 0:128]
        for h in range(4):
            P.op("act", lambda e, h=h: e.activation(out=junk, in_=po[0:n, h * 128:(h + 1) * 128], func=AF.Square,
                                                    accum_out=ss[0:n, h:h + 1]),
                 reads=[PB[pb]], writes=[tmp_t[0], tss])
        sq, tsq = scratch()
        P.op("act", lambda e: e.activation(out=sq[0:n, :], in_=ss[0:n, :], func=AF.Sqrt, scale=1.0 / HD, bias=EPS),
             reads=[tss], writes=[tsq])
        P.op("dve", lambda e: e.reciprocal(out=ss[0:n, :], in_=sq[0:n, :]), reads=[tsq], writes=[tss])
        kn = tmpv[1][0:n, :]
        kn3 = kn.rearrange("p (h d) -> p h d", d=128)
        for h in range(4):
            if on_dve:
                P.op("dve", lambda e, h=h: e.tensor_scalar(out=kn[:, h * 128:(h + 1) * 128],
                                                           in0=po[0:n, h * 128:(h + 1) * 128],
                                                           scalar1=ss[0:n, h:h + 1], scalar2=None, op0=ALU.mult),
                     reads=[PB[pb], tss], writes=[tmp_t[1]])
            else:
                P.op("act", lambda e, h=h: e.activation(out=kn[:, h * 128:(h + 1) * 128],
                                                        in_=po[0:n, h * 128:(h + 1) * 128],
                                                        func=AF.Identity, scale=ss[0:n, h:h + 1]),
                     reads=[PB[pb], tss], writes=[tmp_t[1]])
        P.op("dve", lambda e: e.tensor_tensor(out=kn3, in0=kn3, in1=g_b[0:n, :].unsqueeze(1).to_broadcast([n, 4, 128]),
                                              op=ALU.mult), reads=[tmp_t[1], t_const], writes=[tmp_t[1]])
        t1 = tmpv[2][0:n, :].rearrange("p (h d) -> p h d", d=128)
        t2 = tmpv[3][0:n, :].rearrange("p (h a b c) -> p h a b c", a=2, b=2, c=32)
        kn5 = kn.rearrange("p (h a b c) -> p h a b c", a=2, b=2, c=32)
        cs2 = tab[:, 128:256].rearrange("p (a b c) -> p a b c", a=2, b=2, c=32)
        P.op("dve", lambda e: e.tensor_tensor(out=t1, in0=kn3, in1=tab[:, 0:128].unsqueeze(1).to_broadcast([n, 4, 128]),
                                              op=ALU.mult), reads=[tmp_t[1], tt], writes=[tmp_t[2]])
        for bsel in range(2):
            P.op("dve", lambda e, bsel=bsel: e.tensor_tensor(
                out=t2[:, :, :, bsel, :], in0=kn5[:, :, :, 1 - bsel, :],
                in1=cs2[:, :, bsel, :].unsqueeze(1).to_broadcast([n, 4, 2, 32]), op=ALU.mult),
                reads=[tmp_t[1], tt], writes=[tmp_t[3]])
        P.op("dve", lambda e: e.tensor_tensor(out=kb, in0=tmpv[2][0:n, :], in1=tmpv[3][0:n, :], op=ALU.add),
             reads=[tmp_t[2], tmp_t[3]], writes=[kbf_t[i]])
        if evac_to_T is None:
            return kb, kbf_t[i]
        transpose_heads(kb, n, kbf_t[i], evac_to_T)

    def run_pipeline(tiles):
        n_ = len(tiles)

        def a1(i_):
            if 0 <= i_ < n_ and tiles[i_][0] is not None:
                tiles[i_][0][0]()

        def a2(i_):
            if 0 <= i_ < n_ and tiles[i_][0] is not None:
                tiles[i_][0][1]()
        a1(0)
        a1(1)
        a2(0)
        for i_ in range(n_):
            a1(i_ + 2)
            a2(i_ + 1)
            tiles[i_][1]()
            if i_ > 0:
                tiles[i_ - 1][2]()
        if n_:
            tiles[n_ - 1][2]()

    ACCO = BIG
    ACCZ = BIG + 16384
    QAT = BIG + 32768
    KAT = QAT + 8192
    VA = KAT + 8192
    PTA = VA + 8192
    EXA = PTA + 2048
    accO = V(ACCO, [128, 4, 1024], F32); accZ = V(ACCZ, [128, 4, 1024], F32)
    t_accO = T("accO"); t_accZ = T("accZ")
    qaT = V(QAT, [128, 4, 1024]); t_qaT = [T("qaT%d" % i) for i in range(8)]
    pta_t = [T("pta0"), T("pta1")]; exa_t = [T("exa0"), T("exa1")]
    alias([t_accO, t_accZ] + t_qaT + pta_t + exa_t, [t_acc] + stg_t)

    units = []
    for u in range(4):
        units.append((2, [(cl, 0, 1) for cl in range(4 * u, 4 * u + 4)]))
    for u in range(2):
        units.append((1, [(cl, 0, 2) for cl in (2 * u, 2 * u + 1)]))
    units.append((0, [(0, 0, 4)]))
    units.append((0, [(0, 4, 8)]))

    prev_kv_tiles = []
    cur_g = None
    for (gi, items) in units:
        r = A_DIL[gi]
        nq = 1024 // r
        qb = min(nq, 128)
        nkt = nq // qb + 1
        kvalid = (kv0, kv1, kv2)[gi]

        def ktsz(m, gi=gi):
            return 64 if (gi == 2 and m == 1) else 128
        if cur_g != gi:
            cur_g = gi
            wq, wq_t = wload(w_cols(w_in, gi * 1536, 512), [128, 16, 512])
            wk, wk_t = wload(w_cols(w_in, gi * 1536 + 512, 512), [128, 16, 512])
            wv, wv_t = wload(w_cols(w_in, gi * 1536 + 1024, 512), [128, 16, 512])
            alias(t_qaT, prev_kv_tiles)
            qtiles = []
            for j in range(8):
                st_ = {}

                def evq(tp, pt, j=j, r=r, nq=nq):
                    w = 128 // r
                    qtmp = V(TMP + 3 * 2048, [128, 512]); tq_ = tmp_t[3]
                    P.op("act", lambda e: e.activation(out=qtmp, in_=tp.rearrange("p h n -> p (h n)"), func=AF.Identity),
                         reads=[pt], writes=[tq_])
                    dstv = qaT.rearrange("p h (c u) -> p h c u", u=nq)[:, :, :, j * w:(j + 1) * w]
                    srcv = qtmp.rearrange("p (h u c) -> p h c u", h=4, c=r)
                    P.op("dve", lambda e: e.tensor_copy(out=dstv, in_=srcv), reads=[tq_], writes=[t_qaT[j]])

                def Bq(j=j, st_=st_, wq=wq, wq_t=wq_t):
                    pb_ = 4 + (j % 2)
                    po = proj_tm(hT_own[:, :, j * 128:(j + 1) * 128], 128, [hto_t[j]], wq, wq_t, pb_)
                    tab, tt = load_tab(tabA_d[1024 + j * 128:1024 + (j + 1) * 128, :], 128, 64)
                    st_["kb"] = rope_A(po, 128, pb_, tab, tt)

                def Cq(st_=st_, evq=evq):
                    kb, kbt = st_["kb"]
                    transpose_heads(kb, 128, kbt, evq)
                qtiles.append((None, Bq, Cq))
            run_pipeline(qtiles)
        if stop_after == 'q':
            dd = V(ACCO, [128, 2048], F32); t_dd = T('dd')
            P.op('dve', lambda e: e.tensor_copy(out=dd, in_=qaT[:, 0:2, :]) if False else e.tensor_copy(out=dd.rearrange('p (a b) -> p a b', b=1024), in_=qaT[:, 0:2, :]), reads=t_qaT, writes=[t_dd])
            return emit((dd, [t_dd]))
        nit = len(items)
        kstride = max((b_hi - b_lo + 1) for (_, b_lo, b_hi) in items) * 128
        kaT = V(KAT, [128, 4, nit * kstride])
        t_ka = [T("ka%d" % i) for i in range(nit)]
        t_va = [T("va%d" % i) for i in range(nit)]
        alias(t_ka + t_va, prev_kv_tiles)
        K0 = nq - 64 if r == 1 else (1024 // r) - 64
        xr_c = xr.rearrange("(u c) d -> c u d", c=r)
        tabA_c = tabA_d.rearrange("(u c) d -> c u d", c=r)
        vslot = {}
        sl = 0
        ktiles = []
        tix = 0
        for ii, (cl, b_lo, b_hi) in enumerate(items):
            for m in range(b_lo, b_hi + 1):
                n = ktsz(m)
                u0 = K0 + 128 * m
                kpos = ii * kstride + (m - b_lo) * 128
                vv = V(VA + sl * 1024, [128, 512])[0:n]
                sl += 1
                vslot[(ii, m)] = vv
                st_ = {}

                def Ak1(st_=st_, cl=cl, u0=u0, n=n):
                    st_["a1"] = produce_a1(xr_c[cl, u0:u0 + n, :], n)

                def Ak2(st_=st_):
                    st_["h"] = produce_htt_a2(st_["a1"])

                def Bk(st_=st_, cl=cl, u0=u0, n=n, tix=tix, vv=vv, ii=ii, wk=wk, wk_t=wk_t, wv=wv, wv_t=wv_t, t_va=t_va):
                    htt, htt_tile = st_["h"]
                    pb_ = 4 + (tix % 2)
                    pk = proj_tm(htt, n, [htt_tile], wk, wk_t, pb_)
                    pv = proj_tm(htt, n, [htt_tile], wv, wv_t, 7)
                    tab, tt = load_tab(tabA_c[cl, u0:u0 + n, :], n, 64)
                    st_["kb"] = rope_A(pk, n, pb_, tab, tt)
                    P.op("act", lambda e: e.activation(out=vv, in_=pv[0:n, :], func=AF.Identity),
                         reads=[PB[7]], writes=[t_va[ii]])

                def Ck(st_=st_, kpos=kpos, n=n, ii=ii, kaT=kaT, t_ka=t_ka):
                    kb, kbt = st_["kb"]

                    def evk(tp, pt):
                        for h in range(4):
                            P.op("dve", lambda e, h=h: e.tensor_copy(out=kaT[:, h, kpos:kpos + n], in_=tp[:, h, 0:n]),
                                 reads=[pt], writes=[t_ka[ii]])
                    transpose_heads(kb, n, kbt, evk)
                ktiles.append(((Ak1, Ak2), Bk, Ck))
                tix += 1
        run_pipeline(ktiles)
        if stop_after == 'kv':
            dd = V(ACCO, [128, 2048], F32); t_dd = T('dd')
            P.op('dve', lambda e: e.tensor_copy(out=dd[:, 0:1024], in_=kaT[:, 0, 0:1024]), reads=t_ka, writes=[t_dd])
            P.op('dve', lambda e: e.tensor_copy(out=dd[:, 1024:2048], in_=V(VA, [128, 1024])), reads=t_va, writes=[t_dd])
            return emit((dd, [t_dd]))
        steps = []
        for ii, (cl, b_lo, b_hi) in enumerate(items):
            for h in range(4):
                for m in range(b_lo, b_hi + 1):
                    steps.append((ii, cl, b_lo, b_hi, h, m))

        def S_step(k, steps=steps, kaT=kaT, t_ka=t_ka, kvalid=kvalid, qb=qb, nq=nq, nkt=nkt, kstride=kstride, ktsz=ktsz):
            ii, cl, b_lo, b_hi, h, m = steps[k]
            n = ktsz(m)
            blocks = [bq for bq in (m - 1, m) if b_lo <= bq < b_hi]
            c_lo = blocks[0] * qb
            ncols = len(blocks) * qb
            bi = k % 2
            sb = pbank(bi)
            kpos = ii * kstride + (m - b_lo) * 128
            qcol0 = cl * nq
            P.op("pe", lambda e: e.matmul(sb[0:n, 0:ncols], lhsT=kaT[:, h, kpos:kpos + n],
                                          rhs=qaT[:, h, qcol0 + c_lo:qcol0 + c_lo + ncols], start=True, stop=True),
                 reads=[t_ka[ii]] + t_qaT, writes=[PB[bi]])
            ex = V(EXA + bi * 1024, [128, 512])[0:n, 0:ncols]
            pt_ = V(PTA + bi * 1024, [128, 512])[0:n, 0:ncols]
            P.op("act", lambda e: e.activation(out=ex, in_=sb[0:n, 0:ncols], func=AF.Exp, scale=SCALE),
                 reads=[PB[bi]], writes=[exa_t[bi]])
            if len(blocks) == 2:
                mk = mask2[0:n, 0:256]
            elif blocks[0] == m:
                mk = mask2[0:n, 128:128 + qb]
            else:
                mk = mask2[0:n, 0:qb]
            kvc = cl * nkt + m
            P.op("dve", lambda e: e.scalar_tensor_tensor(out=pt_, in0=ex, scalar=kvalid[0:n, kvc:kvc + 1], in1=mk,
                                                         op0=ALU.mult, op1=ALU.mult),
                 reads=[exa_t[bi], t_const], writes=[pta_t[bi]])
            return (pt_, blocks, c_lo, n, bi)

        def PV_step(k, info, steps=steps, vslot=vslot, t_va=t_va, qb=qb, r=r, gi=gi):
            ii, cl, b_lo, b_hi, h, m = steps[k]
            pt_, blocks, c_lo, n, bi = info
            ob = 2 + (h % 2)
            zb = 4 + (h % 2)
            vv = vslot[(ii, m)]

            def g(e):
                ins = None
                for bq in blocks:
                    col = bq * qb - c_lo
                    first = (m == bq)
                    last = (m == bq + 1)
                    po_ = pbank(ob)[:, (bq - b_lo) * qb:(bq - b_lo + 1) * qb]
                    pz_ = pbank(zb)[:, (bq - b_lo) * qb:(bq - b_lo + 1) * qb]
                    e.matmul(po_, lhsT=vv[:, h * 128:(h + 1) * 128], rhs=pt_[:, col:col + qb],
                             start=first, stop=last, skip_group_check=True)
                    ins = e.matmul(pz_, lhsT=ones_bf[0:n, :], rhs=pt_[:, col:col + qb], start=first,
                                   stop=last, skip_group_check=True)
                return ins
            P.op("pe", g, reads=[pta_t[bi], t_va[ii], t_const], writes=[PB[ob], PB[zb]])
            if m == b_hi:
                nqc = (b_hi - b_lo) * qb
                po_all = pbank(ob)[:, 0:nqc]
                pz_all = pbank(zb)[:, 0:nqc]
                dO = accO[:, h, :].rearrange("p (u c) -> p c u", c=r)[:, cl, b_lo * qb:b_hi * qb]
                dZ = accZ[:, h, :].rearrange("p (u c) -> p c u", c=r)[:, cl, b_lo * qb:b_hi * qb]
                eo = tmpv[0][:, 0:nqc]
                ez = tmpv[1][:, 0:nqc]
                P.op("act", lambda e: e.activation(out=eo, in_=po_all, func=AF.Identity),
                     reads=[PB[ob]], writes=[tmp_t[0]])
                P.op("act", lambda e: e.activation(out=ez, in_=pz_all, func=AF.Identity),
                     reads=[PB[zb]], writes=[tmp_t[1]])
                if gi == 2:
                    P.op("dve", lambda e: e.tensor_copy(out=dO, in_=eo), reads=[tmp_t[0]], writes=[t_accO])
                    P.op("dve", lambda e: e.tensor_copy(out=dZ, in_=ez), reads=[tmp_t[1]], writes=[t_accZ])
                else:
                    P.op("dve", lambda e: e.tensor_tensor(out=dO, in0=dO, in1=eo, op=ALU.add),
                         reads=[tmp_t[0]], writes=[t_accO])
                    P.op("dve", lambda e: e.tensor_tensor(out=dZ, in0=dZ, in1=ez, op=ALU.add),
                         reads=[tmp_t[1]], writes=[t_accZ])
        infos = {0: S_step(0)}
        for k in range(len(steps)):
            if k + 1 < len(steps):
                infos[k + 1] = S_step(k + 1)
            PV_step(k, infos[k])
        prev_kv_tiles = t_ka + t_va + pta_t + exa_t
        if stop_after == 'u0':
            return emit((accO[:, 0, :], [t_accO, t_accZ]))

    oaT = V(OATA, [128, 4, 1024]); t_oaT = T("oaT")
    accOf = V(ACCO, [128, 4096], F32); accZf = V(ACCZ, [128, 4096], F32)
    P.op("dve", lambda e: e.reciprocal(out=accZf, in_=accZf), reads=[t_accZ], writes=[t_accZ])
    P.op("dve", lambda e: e.tensor_tensor(out=V(OATA, [128, 4096]), in0=accOf, in1=accZf, op=ALU.mult),
         reads=[t_accO, t_accZ], writes=[t_oaT])

    if stop_after == 'p1b':
        dd = V(BIG, [128, 2048], F32)
        t_dd = T('dd')
        alias([t_dd], [t_accO, t_accZ])
        P.op('dve', lambda e: e.tensor_copy(out=dd, in_=V(OATA, [128, 4096])[:, 0:2048]), reads=[t_oaT], writes=[t_dd])
        return emit((dd, [t_dd]))
    ring_n[0] = 2
    ring_i[0] = 0
    kbT = V(BIG, [128, 4, 4096]); vB = V(BIG + 32768, [128, 32, 512])
    t_kb = [T("kb%d" % j) for j in range(32)]
    t_vb = [T("vb%d" % j) for j in range(32)]
    alias(t_kb + t_vb, [t_accO, t_accZ] + t_qaT + prev_kv_tiles)
    wkB, wkB_t = wload(w_cols(w_in, 4608 + 2048, 512), [128, 16, 512])
    wvB, wvB_t = wload(w_cols(w_in, 4608 + 2560, 512), [128, 16, 512])
    kvtiles = []
    for j in range(32):
        st_ = {}
        own = 8 <= j < 16

        def Ab1(st_=st_, j=j):
            st_["a1"] = produce_a1(xr[j * 128:(j + 1) * 128, :], 128)

        def Ab2(st_=st_):
            st_["h"] = produce_htt_a2(st_["a1"])

        def Bb(st_=st_, j=j, own=own):
            if own:
                h_ap = hT_own[:, :, (j - 8) * 128:(j - 7) * 128]
                h_t = [hto_t[j - 8]]
            else:
                h_ap, ht_ = st_["h"]
                h_t = [ht_]
            pb_ = 4 + (j % 2)
            pk = proj_tm(h_ap, 128, h_t, wkB, wkB_t, pb_)
            pv = proj_tm(h_ap, 128, h_t, wvB, wvB_t, 7)
            tab, tt = load_tab(tabB_d[j * 128:(j + 1) * 128, :], 128, 256)
            st_["kb"] = rope_B(pk, 128, pb_, tab, tt, kg_b)
            P.op("act", lambda e: e.activation(out=vB[:, j, :], in_=pv, func=AF.Identity),
                 reads=[PB[7]], writes=[t_vb[j]])

        def Cb(st_=st_, j=j):
            kb, kbt = st_["kb"]

            def evk(tp, pt):
                for h in range(4):
                    P.op("dve", lambda e, h=h: e.tensor_copy(out=kbT[:, h, j * 128:(j + 1) * 128], in_=tp[:, h, :]),
                         reads=[pt], writes=[t_kb[j]])
            transpose_heads(kb, 128, kbt, evk)
        kvtiles.append((None if own else (Ab1, Ab2), Bb, Cb))
    htmp_addr[0] = RING_ADDR[2]
    alias([t_htmp], [ring_t[2]] + prev_kv_tiles + [t_accO, t_accZ] + t_qaT)
    run_pipeline(kvtiles)
    alias([ring_t[2]], [t_htmp])

    ring_n[0] = 1
    ring_i[0] = 0
    QB_ADDR = [RING_ADDR[2], RING_ADDR[1]]
    PTB = RING_ADDR[2] + 8192
    EPB = PTB + 4096
    qbTs = [V(a_, [128, 4, 1024]) for a_ in QB_ADDR]
    t_qbs = [[T("qb%d_%d" % (s_, i)) for i in range(8)] for s_ in range(2)]
    ptb = [V(PTB + i * 1024, [128, 512]) for i in range(4)]
    ptb_t = [T("ptb%d" % i) for i in range(4)]
    epb = V(EPB, [128, 512], F32); t_epb = T("epb")
    alias(t_qbs[0] + ptb_t + [t_epb], [ring_t[2]])
    alias(t_qbs[1], [ring_t[1]])
    t_qb = t_qbs[0] + t_qbs[1]
    obT = V(XT, [128, 16, 1024])
    t_ob = [T("ob%d" % i) for i in range(32)]
    alias(t_ob, xt_t + xn_t + htt_t)

    def q_tiles(kvh):
        wqB, wqB_t = wload(w_cols(w_in, 4608 + kvh * 512, 512), [128, 16, 512])
        qbT = qbTs[kvh % 2]
        tq = t_qbs[kvh % 2]
        tl = []
        for jt in range(8):
            st_ = {}

            def Bq(st_=st_, jt=jt):
                pb_ = 6
                pq = proj_tm(hT_own[:, :, jt * 128:(jt + 1) * 128], 128, [hto_t[jt]], wqB, wqB_t, pb_)
                tab, tt = load_tab(tabB_d[1024 + jt * 128:1024 + (jt + 1) * 128, :], 128, 256)
                st_["kb"] = rope_B(pq, 128, pb_, tab, tt, qg_b, on_dve=True)

            def Cq(st_=st_, jt=jt):
                kb, kbt = st_["kb"]

                def evq(tp, pt):
                    for h in range(4):
                        P.op("dve", lambda e, h=h: e.tensor_copy(out=qbT[:, h, jt * 128:(jt + 1) * 128], in_=tp[:, h, :]),
                             reads=[pt], writes=[tq[jt]])
                transpose_heads(kb, 128, kbt, evq, pb=6)
            tl.append((None, Bq, Cq))
        return tl

    def attn(kvh, inter):
        qbT = qbTs[kvh % 2]
        tq = t_qbs[kvh % 2]
        steps = [(hh, half, t) for hh in range(4) for half in range(2) for t in range(32)]

        def issue_S(si):
            hh, half, t = steps[si]
            sbk = (0, 1, 7)[si % 3]
            pi = si % 4
            P.op("pe", lambda e: e.matmul(pbank(sbk), lhsT=kbT[:, kvh, t * 128:(t + 1) * 128],
                                          rhs=qbT[:, hh, half * 512:(half + 1) * 512], start=True, stop=True),
                 reads=[t_kb[t]] + tq[half * 4:half * 4 + 4], writes=[PB[sbk]])
            P.op("act", lambda e: e.activation(out=ptb[pi], in_=pbank(sbk), func=AF.Exp, scale=SCALE),
                 reads=[PB[sbk]], writes=[ptb_t[pi]])

        def issue_PV(si):
            hh, half, t = steps[si]
            pi = si % 4
            sel = (hh * 2 + half) % 2
            ob, zb = 2 + sel, 4 + sel

            def g(e):
                e.matmul(pbank(ob), lhsT=vB[:, t, kvh * 128:(kvh + 1) * 128], rhs=ptb[pi], start=(t == 0),
                         stop=(t == 31))
                return e.matmul(pbank(zb), lhsT=ones_bf, rhs=ptb[pi], start=(t == 0), stop=(t == 31))
            P.op("pe", g, reads=[ptb_t[pi], t_vb[t], t_const], writes=[PB[ob], PB[zb]])
            if t == 31:
                head = kvh * 4 + hh
                P.op("dve", lambda e: e.reciprocal(out=epb, in_=pbank(zb)), reads=[PB[zb]], writes=[t_epb])
                P.op("dve", lambda e: e.tensor_tensor(out=obT[:, head, half * 512:(half + 1) * 512], in0=pbank(ob),
                                                      in1=epb, op=ALU.mult),
                     reads=[PB[ob], t_epb], writes=[t_ob[head * 2 + half]])
        inter_ops = []
        if inter:
            n_ = len(inter)
            for i_ in range(n_):
                inter_ops.append(inter[i_][1])
                if i_ > 0:
                    inter_ops.append(inter[i_ - 1][2])
            inter_ops.append(inter[n_ - 1][2])
        issue_S(0)
        issue_S(1)
        for si in range(len(steps)):
            if si + 2 < len(steps):
                issue_S(si + 2)
            issue_PV(si)
            if si % 14 == 7 and inter_ops:
                inter_ops.pop(0)()
        while inter_ops:
            inter_ops.pop(0)()

    run_pipeline(q_tiles(0))
    for kvh in range(4):
        attn(kvh, q_tiles(kvh + 1) if kvh + 1 < 4 else None)
    if stop_after == 'p2':
        dd = V(BIG, [128, 2048], F32)
        t_dd = T('dd')
        alias([t_dd], t_kb + t_vb)
        P.op('dve', lambda e: e.tensor_copy(out=dd.rearrange('p (a b) -> p a b', b=1024), in_=obT[:, 0:2, :]), reads=t_ob, writes=[t_dd])
        return emit((dd, [t_dd]))
    ring_n[0] = 3
    ring_i[0] = 0
    alias([ring_t[2]], t_qbs[0] + ptb_t + [t_epb])
    alias([ring_t[1]], t_qbs[1])
    mT = V(BIG, [128, 16, 1024]); t_mT = [T("mT%d" % i) for i in range(32)]
    alias(t_mT, t_kb + t_vb)
    G3 = BIG + 32768
    g3v = [V(G3 + i * 2048, [128, 512], F32) for i in range(8)]
    g3t = [T("g3_%d" % i) for i in range(8)]
    alias(g3t, t_kb + t_vb)
    for dc in range(16):
        off, wt = wslot()
        wga = V(off, [128, 16, 128]); wgb = V(off + 4096, [128, 16, 128])
        wbu = V(off + 8192, [128, 16, 128]); wau = V(off + 12288, [128, 4, 128])
        P.dma("pool", "dw", wga, w_cols(w_in, 7680 + dc * 128, 128), writes=[wt])
        P.dma("pool", "dw", wgb, w_cols(w_in, 7680 + 2048 + dc * 128, 128), writes=[wt])
        P.dma("pool", "dw", wbu, w_cols(w_b_up, dc * 128, 128), writes=[wt])
        P.dma("pool", "dw", wau, w_cols(w_a_up, dc * 128, 128), writes=[wt])
        for half in range(2):
            hs = slice(half * 512, (half + 1) * 512)
            bga, bgb, bya, byb = half, 2 + half, 4 + half, 6 + half

            def fg(e, wsrc, bank, hs=hs):
                ins = None
                for k in range(16):
                    ins = e.matmul(pbank(bank), lhsT=wsrc[:, k, :], rhs=hT_own[:, k, hs], start=(k == 0), stop=(k == 15))
                return ins
            P.op("pe", lambda e, wga=wga, bga=bga, fg=fg: fg(e, wga, bga), reads=[wt] + hto_t[half * 4:half * 4 + 4],
                 writes=[PB[bga]])
            P.op("pe", lambda e, wgb=wgb, bgb=bgb, fg=fg: fg(e, wgb, bgb), reads=[wt] + hto_t[half * 4:half * 4 + 4],
                 writes=[PB[bgb]])

            def fya(e, wau=wau, bya=bya, hs=hs):
                ins = None
                for h in range(4):
                    ins = e.matmul(pbank(bya), lhsT=wau[:, h, :], rhs=oaT[:, h, hs], start=(h == 0), stop=(h == 3))
                return ins
            P.op("pe", fya, reads=[wt, t_oaT], writes=[PB[bya]])

            def fyb(e, wbu=wbu, byb=byb, hs=hs):
                ins = None
                for h in range(16):
                    ins = e.matmul(pbank(byb), lhsT=wbu[:, h, :], rhs=obT[:, h, hs], start=(h == 0), stop=(h == 15))
                return ins
            P.op("pe", fyb, reads=[wt] + [t_ob[h * 2 + half] for h in range(16)], writes=[PB[byb]])
            sga, sgb, t1, t2 = [g3v[half * 4 + i] for i in range(4)]
            tga, tgb, tt1, tt2 = [g3t[half * 4 + i] for i in range(4)]
            P.op("act", lambda e, sga=sga, bga=bga, dc=dc: e.activation(out=sga, in_=pbank(bga), func=AF.Sigmoid,
                                                                        bias=bgate[:, dc:dc + 1]),
                 reads=[PB[bga], t_const], writes=[tga])
            P.op("act", lambda e, sgb=sgb, bgb=bgb, dc=dc: e.activation(out=sgb, in_=pbank(bgb), func=AF.Sigmoid,
                                                                        bias=bgate[:, 16 + dc:17 + dc]),
                 reads=[PB[bgb], t_const], writes=[tgb])
            P.op("dve", lambda e, t1=t1, sga=sga, bya=bya: e.tensor_tensor(out=t1, in0=sga, in1=pbank(bya), op=ALU.mult),
                 reads=[tga, PB[bya]], writes=[tt1])
            P.op("dve", lambda e, t2=t2, sgb=sgb, byb=byb: e.tensor_tensor(out=t2, in0=sgb, in1=pbank(byb), op=ALU.mult),
                 reads=[tgb, PB[byb]], writes=[tt2])
            P.op("dve", lambda e, t1=t1, t2=t2, dc=dc, hs=hs: e.tensor_tensor(out=mT[:, dc, hs], in0=t1, in1=t2,
                                                                              op=ALU.add),
                 reads=[tt1, tt2], writes=[t_mT[dc * 2 + half]])

    def bcast_pl(src_pl, src_t, dst, dst_t, dg_off, dg_alias):
        dg = V(dg_off, [128, 16, 128], F32)
        t_dg = T("dg")
        alias([t_dg], dg_alias)
        for c in range(16):
            P.op("dve", lambda e, c=c: e.tensor_scalar(out=dg[:, c, :], in0=ident_f, scalar1=src_pl[:, c:c + 1],
                                                       scalar2=None, op0=ALU.mult),
                 reads=[t_const, src_t], writes=[t_dg])
        pbv = pbanks(0, 4)

        def f(e):
            ins = None
            for c in range(16):
                ins = e.matmul(pbv[:, c * 128:(c + 1) * 128], lhsT=ones_f, rhs=dg[:, c, :], start=True, stop=True)
            return ins
        P.op("pe", f, reads=[t_dg, t_const], writes=PB[0:4])
        P.op("act", lambda e: e.activation(out=dst, in_=pbv, func=AF.Identity), reads=PB[0:4], writes=[dst_t])

    ring_n[0] = 4
    alias([ring_t[3]], [t_oaT] + tmp_t + kbf_t + tab_t)
    x1 = V(XT, [128, 8, 2048], F32)
    t_x1 = [[T("x1_%d_%d" % (j, c)) for c in range(4)] for j in range(8)]
    old = t_ob + hto_t + xt_t + xn_t + htt_t
    for j in range(8):
        alias(t_x1[j], old)
    gt1b = V(G3, [128, 2048], F32); t_gt1b = T("gt1b")
    alias([t_gt1b], g3t)
    bcast_pl(modT[:, 32:48], t_modT, gt1b, t_gt1b, G3 + 8192, g3t)
    for j in range(8):
        P.dma("sp", "dx", x1[:, j, :], xr[1024 + j * 128:1024 + (j + 1) * 128, :], writes=t_x1[j])
    ytmp = [V(G3 + 16384 + i * 2048, [128, 512], F32) for i in range(2)]
    ytmp_t = [T("ytmp0"), T("ytmp1")]
    alias(ytmp_t, g3t)
    blk = 0
    for cb in range(4):
        wo, wo_t = wload(w_cols(w_out, cb * 512, 512), [128, 16, 512])
        for j in range(8):
            bank = blk % 4
            yt, ytt = ytmp[blk % 2], ytmp_t[blk % 2]
            blk += 1

            def f(e, bank=bank, j=j, wo=wo):
                ins = None
                for k in range(16):
                    ins = e.matmul(pbank(bank), lhsT=mT[:, k, j * 128:(j + 1) * 128], rhs=wo[:, k, :], start=(k == 0),
                                   stop=(k == 15))
                return ins
            P.op("pe", f, reads=[wo_t] + t_mT[(j // 4)::2], writes=[PB[bank]])
            cs = slice(cb * 512, (cb + 1) * 512)
            P.op("dve", lambda e, yt=yt, bank=bank, cs=cs: e.tensor_tensor(out=yt, in0=pbank(bank), in1=gt1b[:, cs],
                                                                           op=ALU.mult),
                 reads=[PB[bank], t_gt1b], writes=[ytt])
            P.op("dve", lambda e, yt=yt, j=j, cs=cs: e.tensor_tensor(out=x1[:, j, cs], in0=x1[:, j, cs], in1=yt,
                                                                     op=ALU.add),
                 reads=[ytt], writes=[t_x1[j][cb]])

    if stop_after == 'p3':
        return emit((x1[:, 0, :], t_x1[0]))
    h2T = V(BIG, [128, 16, 1024]); t_h2 = [T("h2_%d" % j) for j in range(8)]
    alias(t_h2, t_mT)
    GT2 = G3
    HID = G3 + 4096
    STMP = HID + 8192
    GATE = STMP + 4096
    RT = GATE + 2048
    XN2 = RT + 2048
    WR = XN2 + 8192
    GT2F = HID
    assert WR + 2048 <= ARENA * 2
    gt2b = V(GT2, [128, 2048]); t_gt2b = T("gt2b")
    hidT = V(HID, [128, 4, 1024]); t_hid = [[T("hid%d%d" % (fc, hf)) for hf in range(2)] for fc in range(4)]
    stmp = [V(STMP + i * 2048, [128, 512], F32) for i in range(2)]; stmp_t = [T("st0"), T("st1")]
    gate = V(GATE, [128, 8, 64], F32); t_gate = [T("gate%d" % j) for j in range(8)]
    rt = [V(RT + i * 256, [128, 64], F32) for i in range(8)]; rt_t = [T("rt%d" % i) for i in range(8)]
    xn2 = [V(XN2 + i * 4096, [128, 2048]) for i in range(2)]; xn2_t = [T("xn2_0"), T("xn2_1")]
    wr = V(WR, [128, 16, 64]); t_wr = T("wr")
    gt2f = V(GT2F, [128, 2048], F32); t_gt2f = T("gt2f")
    p4 = [t_gt2b] + [t for l in t_hid for t in l] + stmp_t + t_gate + rt_t + xn2_t + [t_wr, t_gt2f]
    alias(p4, g3t + [t_gt1b] + ytmp_t + t_kb + t_vb)
    P.dma("pool", "dw", wr, w_router.rearrange("(k p) n -> p k n", p=128), writes=[t_wr])
    bcast_pl(modT[:, 80:96], t_modT, gt2f, t_gt2f, XN2, xn2_t)
    P.op("dve", lambda e: e.tensor_copy(out=gt2b, in_=gt2f), reads=[t_gt2f], writes=[t_gt2b])
    alias([t for l in t_hid for t in l], [t_gt2f])
    htmp_addr[0] = HID + 4096
    alias([t_htmp], [t_gt2f])
    p4st = [dict() for _ in range(8)]

    def P4a(j):
        i = j % 2
        rstd, trs = rms_rstd(x1[:, j, :], 128, D, t_x1[j], xn2[i], [xn2_t[i]])
        P.op("dve", lambda e, i=i, j=j, rstd=rstd: e.tensor_scalar(out=xn2[i], in0=x1[:, j, :], scalar1=rstd,
                                                                   scalar2=None, op0=ALU.mult),
             reads=t_x1[j] + [trs], writes=[xn2_t[i]])

    def P4b(j):
        i = j % 2
        transpose_mod(xn2[i], 128, [xn2_t[i]], h2T[:, :, j * 128:(j + 1) * 128], t_h2[j], A2, B2, t_A2, i * 2)

    def P4c(j):
        i = j % 2
        pl = pbank(4 + i)[:, 0:64]

        def fr(e, j=j, pl=pl):
            ins = None
            for k in range(16):
                ins = e.matmul(pl, lhsT=h2T[:, k, j * 128:(j + 1) * 128], rhs=wr[:, k, :], start=(k == 0),
                               stop=(k == 15))
            return ins
        P.op("pe", fr, reads=[t_h2[j], t_wr], writes=[PB[4 + i]])
        routing(j, i, pl)

    def routing(j, i, pl):
            sc_, sel, eq, selm, em = rt[0], rt[1], rt[2], rt[3], rt[4]
            sm = rt[5]
            sel3 = sel.rearrange("p (g e) -> p g e", e=8)
            eq3 = eq.rearrange("p (g e) -> p g e", e=8)
            selm3 = selm.rearrange("p (g e) -> p g e", e=8)
            R = rt_t
            P.op("act", lambda e, pl=pl: e.activation(out=sc_, in_=pl, func=AF.Sigmoid), reads=[PB[4 + i]], writes=[R[0]])
            P.op("dve", lambda e: e.tensor_tensor(out=sel, in0=sc_, in1=ebias_b, op=ALU.add), reads=[R[0], t_const],
                 writes=[R[1]])
            P.op("dve", lambda e: e.tensor_reduce(out=sm[:, 0:8], in_=sel3, axis=AX.X, op=ALU.max), reads=[R[1]],
                 writes=[R[5]])
            P.op("dve", lambda e: e.tensor_tensor(out=eq3, in0=sel3, in1=sm[:, 0:8].unsqueeze(2).to_broadcast([128, 8, 8]),
                                                  op=ALU.is_ge), reads=[R[1], R[5]], writes=[R[2]])
            P.op("dve", lambda e: e.scalar_tensor_tensor(out=selm, in0=eq, scalar=-1e30, in1=sel, op0=ALU.mult,
                                                         op1=ALU.add), reads=[R[2], R[1]], writes=[R[3]])
            P.op("dve", lambda e: e.tensor_reduce(out=sm[:, 8:16], in_=selm3, axis=AX.X, op=ALU.max), reads=[R[3]],
                 writes=[R[5]])
            P.op("dve", lambda e: e.tensor_tensor(out=sm[:, 8:16], in0=sm[:, 8:16], in1=sm[:, 0:8], op=ALU.add),
                 reads=[R[5]], writes=[R[5]])
            P.op("dve", lambda e: e.max(out=sm[:, 16:24], in_=sm[:, 8:16]), reads=[R[5]], writes=[R[5]])
            P.op("dve", lambda e: e.tensor_scalar(out=sm[:, 24:32], in0=sm[:, 8:16], scalar1=sm[:, 19:20], scalar2=None,
                                                  op0=ALU.is_ge), reads=[R[5]], writes=[R[5]])
            P.op("dve", lambda e: e.tensor_scalar(out=sm[:, 24:32], in0=sm[:, 24:32], scalar1=1e30, scalar2=-1e30,
                                                  op0=ALU.mult, op1=ALU.add), reads=[R[5]], writes=[R[5]])
            P.op("dve", lambda e: e.tensor_tensor(out=selm3, in0=sel3,
                                                  in1=sm[:, 24:32].unsqueeze(2).to_broadcast([128, 8, 8]), op=ALU.add),
                 reads=[R[1], R[5]], writes=[R[3]])
            P.op("dve", lambda e: e.max(out=sm[:, 32:40], in_=selm), reads=[R[3]], writes=[R[5]])
            P.op("dve", lambda e: e.tensor_scalar(out=em, in0=selm, scalar1=sm[:, 39:40], scalar2=None, op0=ALU.is_ge),
                 reads=[R[3], R[5]], writes=[R[4]])
            P.op("dve", lambda e: e.tensor_tensor(out=em, in0=em, in1=sc_, op=ALU.mult), reads=[R[4], R[0]],
                 writes=[R[4]])
            P.op("dve", lambda e: e.tensor_reduce(out=sm[:, 40:41], in_=em, axis=AX.X, op=ALU.add), reads=[R[4]],
                 writes=[R[5]])
            P.op("dve", lambda e: e.reciprocal(out=sm[:, 41:42], in_=sm[:, 40:41]), reads=[R[5]], writes=[R[5]])
            P.op("dve", lambda e, j=j: e.tensor_scalar(out=gate[:, j, :], in0=em, scalar1=sm[:, 41:42], scalar2=2.5,
                                                       op0=ALU.mult, op1=ALU.mult), reads=[R[4], R[5]],
                 writes=[t_gate[j]])

    P4a(0)
    P4a(1)
    P4b(0)
    for j in range(8):
        if j + 2 < 8:
            P4a(j + 2)
        if j + 1 < 8:
            P4b(j + 1)
        P4c(j)
    alias([t for l in t_hid for t in l], [t_htmp])
    if stop_after == 'p4r':
        dd = V(XN2, [128, 2048], F32)
        t_dd = T('dd')
        alias([t_dd], xn2_t)
        P.op('dve', lambda e: e.tensor_copy(out=dd[:, 0:512], in_=gate.rearrange('p a b -> p (a b)')), reads=t_gate, writes=[t_dd])
        P.op('dve', lambda e: e.tensor_copy(out=dd[:, 512:1024].rearrange('p (a b) -> p a b', b=128), in_=h2T[:, 0:4, 0:128]), reads=t_h2, writes=[t_dd])
        return emit((dd[:, 0:1024], [t_dd]))
    yblk = [0]
    for ex_i in range(NE + (1 if stop_after != 'noshared' else 0)):
        if ex_i < NE:
            s1, s3, s2 = w1[ex_i], w3[ex_i], w2[ex_i]
        else:
            s1, s3, s2 = ws1, ws3, ws2
        w1v, w1t = wload(s1.rearrange("(k p) n -> p k n", p=128), [128, 16, 512])
        w3v, w3t = wload(s3.rearrange("(k p) n -> p k n", p=128), [128, 16, 512])
        w2v, w2t = wload(s2.rearrange("(f p) n -> p f n", p=128), [128, 4, 2048])
        P.op("dve", lambda e, w2v=w2v: e.tensor_tensor(out=w2v, in0=w2v,
                                                       in1=gt2b.unsqueeze(1).to_broadcast([128, 4, 2048]), op=ALU.mult),
             reads=[w2t, t_gt2b], writes=[w2t])
        u = 0
        for fc in range(4):
            for half in range(2):
                ba, bu = 2 * (u % 2), 2 * (u % 2) + 1
                hs = slice(half * 512, (half + 1) * 512)

                def fup(e, wsrc, bank, fc=fc, hs=hs):
                    ins = None
                    for k in range(16):
                        ins = e.matmul(pbank(bank), lhsT=wsrc[:, k, fc * 128:(fc + 1) * 128], rhs=h2T[:, k, hs],
                                       start=(k == 0), stop=(k == 15))
                    return ins
                P.op("pe", lambda e, fup=fup, w1v=w1v, ba=ba: fup(e, w1v, ba), reads=[w1t] + t_h2[half * 4:half * 4 + 4],
                     writes=[PB[ba]])
                P.op("pe", lambda e, fup=fup, w3v=w3v, bu=bu: fup(e, w3v, bu), reads=[w3t] + t_h2[half * 4:half * 4 + 4],
                     writes=[PB[bu]])
                st, stt = stmp[u % 2], stmp_t[u % 2]
                P.op("act", lambda e, st=st, ba=ba: e.activation(out=st, in_=pbank(ba), func=AF.Silu),
                     reads=[PB[ba]], writes=[stt])
                P.op("dve", lambda e, st=st, bu=bu, fc=fc, hs=hs: e.tensor_tensor(out=hidT[:, fc, hs], in0=st,
                                                                                  in1=pbank(bu), op=ALU.mult),
                     reads=[stt, PB[bu]], writes=[t_hid[fc][half]])
                u += 1
        for j in range(8):
            for cb in range(4):
                bank = 4 + (yblk[0] % 4)
                yblk[0] += 1
                cs = slice(cb * 512, (cb + 1) * 512)

                def fdn(e, bank=bank, j=j, cs=cs, w2v=w2v):
                    ins = None
                    for fc in range(4):
                        ins = e.matmul(pbank(bank), lhsT=hidT[:, fc, j * 128:(j + 1) * 128], rhs=w2v[:, fc, cs],
                                       start=(fc == 0), stop=(fc == 3))
                    return ins
                P.op("pe", fdn, reads=[w2t] + [t_hid[fc][j // 4] for fc in range(4)], writes=[PB[bank]])
                if ex_i < NE:
                    P.op("dve", lambda e, bank=bank, j=j, cs=cs, ex_i=ex_i: e.scalar_tensor_tensor(
                        out=x1[:, j, cs], in0=pbank(bank), scalar=gate[:, j, ex_i:ex_i + 1], in1=x1[:, j, cs],
                        op0=ALU.mult, op1=ALU.add), reads=[PB[bank], t_gate[j]], writes=[t_x1[j][cb]])
                else:
                    P.op("dve", lambda e, bank=bank, j=j, cs=cs: e.tensor_tensor(
                        out=x1[:, j, cs], in0=x1[:, j, cs], in1=pbank(bank), op=ALU.add),
                        reads=[PB[bank]], writes=[t_x1[j][cb]])

    gfin = gt2f
    alias([t_gt2f], [t for l in t_hid for t in l])
    P.dma("sp", "dc", gfin, gfin_d[0].partition_broadcast(128), writes=[t_gt2f])
    for j in range(8):
        i = 0
        rstd, trs = rms_rstd(x1[:, j, :], 128, D, t_x1[j], xn2[i], [xn2_t[i]])
        P.op("dve", lambda e, j=j, rstd=rstd: e.scalar_tensor_tensor(out=x1[:, j, :], in0=x1[:, j, :], scalar=rstd,
                                                                     in1=gfin, op0=ALU.mult, op1=ALU.mult),
             reads=[trs, t_gt2f], writes=t_x1[j])
        P.dma("sp", "do", out_d[j * 128:(j + 1) * 128, :], x1[:, j, :], reads=t_x1[j])
    return emit()


def _host_inputs(inputs):
    x = np.asarray(inputs["x"], np.float32)
    c = np.asarray(inputs["c"], np.float32)

    def pl(v):
        v = np.asarray(v, np.float32).reshape(-1, 128)
        return np.ascontiguousarray(v.T)

    shared = {
        "w_ada": np.ascontiguousarray(np.asarray(inputs["w_ada"], np.float32)[0]),
        "bada_pl": pl(np.asarray(inputs["b_ada"])[0]),
        "gattn_pl": pl(np.asarray(inputs["g_attn"])[0]),
        "gffn_pl": pl(np.asarray(inputs["g_ffn"])[0]),
        "w_in": np.ascontiguousarray(np.asarray(inputs["w_in"], np.float32)[0]),
        "bgate_pl": pl(np.asarray(inputs["b_gate"])[0]),
        "qg": np.asarray(inputs["q_norm_g"], np.float32).reshape(1, HD),
        "kg": np.asarray(inputs["k_norm_g"], np.float32).reshape(1, HD),
        "w_a_up": np.ascontiguousarray(np.asarray(inputs["w_a_up"], np.float32)[0]),
        "w_b_up": np.ascontiguousarray(np.asarray(inputs["w_b_up"], np.float32)[0]),
        "w_out": np.ascontiguousarray(np.asarray(inputs["w_out"], np.float32)[0]),
        "w_router": np.ascontiguousarray(np.asarray(inputs["w_router"], np.float32)[0]),
        "e_bias": np.asarray(inputs["e_bias"], np.float32).reshape(1, NE),
        "w1": np.ascontiguousarray(np.asarray(inputs["w1"], np.float32)[0]),
        "w3": np.ascontiguousarray(np.asarray(inputs["w3"], np.float32)[0]),
        "w2": np.ascontiguousarray(np.asarray(inputs["w2"], np.float32)[0]),
        "ws1": np.ascontiguousarray(np.asarray(inputs["ws1"], np.float32)[0]),
        "ws3": np.ascontiguousarray(np.asarray(inputs["ws3"], np.float32)[0]),
        "ws2": np.ascontiguousarray(np.asarray(inputs["ws2"], np.float32)[0]),
        "g_final": np.asarray(inputs["g_final"], np.float32).reshape(1, D),
        "ident": np.eye(128, dtype=np.float32),
    }
    ii = np.arange(128)[:, None]
    cc = np.arange(128)[None, :]
    shared["mask2"] = np.concatenate([(cc >= ii), (cc <= ii)], axis=1).astype(np.float32)
    pos = np.arange(S, dtype=np.float32)
    invA = np.power(np.float32(500000.0), -np.arange(0, 32, 2, dtype=np.float32) / 32).astype(np.float32)
    angA = pos[:, None] * invA[None, :]
    cA, sA = np.cos(angA).astype(np.float32), np.sin(angA).astype(np.float32)
    tabA = np.concatenate([cA, cA, -sA, sA], axis=1).astype(np.float32)
    invB = np.power(np.float32(10000.0), -np.arange(0, 64, 2, dtype=np.float32) / 64).astype(np.float32)
    row = (np.arange(S) // 64).astype(np.float32)
    col = (np.arange(S) % 64).astype(np.float32)
    ar, ac = row[:, None] * invB[None, :], col[:, None] * invB[None, :]
    cr, sr, cc_, sc_ = np.cos(ar), np.sin(ar), np.cos(ac), np.sin(ac)
    tabB = np.concatenate([cr, cr, cc_, cc_, -sr, sr, -sc_, sc_], axis=1).astype(np.float32)
    in_maps = []
    for core in range(8):
        b, q = core // 4, core % 4
        s0 = q * 1024
        shift = s0 - 1024
        tok = (np.arange(S) + shift) % S
        m = dict(shared)
        m["xr"] = np.ascontiguousarray(x[b][tok])
        m["c_pl"] = pl(c[b])
        m["tabA"] = np.ascontiguousarray(tabA[tok])
        m["tabB"] = np.ascontiguousarray(tabB[tok])
        kvs = []
        for gi, r in enumerate(A_DIL):
            nq = 1024 // r
            qb = min(nq, 128)
            nkt = nq // qb + 1
            K0 = 1024 // r - 64
            kv = np.zeros((128, r * nkt), np.float32)
            for cl in range(r):
                for mm in range(nkt):
                    j = cl + r * (K0 + 128 * mm + np.arange(128))
                    t = j + shift
                    kv[:, cl * nkt + mm] = ((t >= 0) & (t < S) & (j < S)).astype(np.float32)
            kvs.append(kv)
        m["kv0"], m["kv1"], m["kv2"] = kvs
        in_maps.append(m)
    return in_maps


_NC_CACHE = {}


def kernel(**inputs):
    in_maps = _host_inputs(inputs)
    if "nc" not in _NC_CACHE:
        _NC_CACHE["nc"] = build_nc()
    nc = _NC_CACHE["nc"]
    res = run_bass_kernel_spmd(nc, in_maps, core_ids=list(range(8)))
    out = np.zeros((2, S, D), np.float32)
    for core in range(8):
        b, q = core // 4, core % 4
        out[b, q * 1024:(q + 1) * 1024] = res.results[core]["out"]
    return out
```

```python
import contextlib
import numpy as np
import concourse.bass as bass
import concourse.mybir as mybir
from concourse.bass_utils import run_bass_kernel_spmd

F32, BF16 = mybir.dt.float32, mybir.dt.bfloat16
AF = mybir.ActivationFunctionType
ALU = mybir.AluOpType
AX = mybir.AxisListType

D = 2048
S = 4096
NT = 1024
HD = 128
EPS = 1e-6
NE = 64
IN_COLS = 11776
A_DIL = (1, 4, 16)
ARENA = 103200
SCALE = float(HD) ** -0.5


class T:
    __slots__ = ("w", "r", "name")

    def __init__(self, name=""):
        self.w = None
        self.r = {}
        self.name = name


def alias(new_tiles, old_tiles):
    evs = {}
    for o in old_tiles:
        if o.w is not None:
            k, v = o.w
            evs[k] = max(evs.get(k, 0), v)
        for k, v in o.r.items():
            evs[k] = max(evs.get(k, 0), v)
    for n in new_tiles:
        for k, v in evs.items():
            if n.r.get(k, 0) < v:
                n.r[k] = v


class StopBuild(Exception):
    pass


class Prog:
    ENG = ("pe", "act", "dve", "pool", "sp")

    def __init__(self):
        self.q = {e: [] for e in self.ENG}
        self.cnt = {}
        self.waited = {e: {} for e in self.ENG}
        self.dpool_i = {}

    def _waits(self, eng, reads, writes):
        waits = {}

        def need(k, v):
            if waits.get(k, 0) < v:
                waits[k] = v
        for t in reads:
            if t.w is not None:
                need(*t.w)
        for t in writes:
            if t.w is not None:
                need(*t.w)
            for k, v in t.r.items():
                need(k, v)
        wl = []
        for k, v in waits.items():
            if k == eng and eng == "pe":
                continue
            if self.waited[eng].get(k, 0) >= v:
                continue
            self.waited[eng][k] = v
            wl.append((k, v))
        return wl

    def _commit(self, ev, reads, writes):
        k, v = ev
        for t in reads:
            if t.r.get(k, 0) < v:
                t.r[k] = v
        for t in writes:
            t.w = ev
            t.r = {}

    def op(self, eng, fn, reads=(), writes=()):
        reads = list(reads)
        writes = list(writes)
        wl = self._waits(eng, reads, writes)
        self.cnt[eng] = self.cnt.get(eng, 0) + 1
        ev = (eng, self.cnt[eng])
        self.q[eng].append((wl, fn, (eng, 1)))
        self._commit(ev, reads, writes)
        return ev

    NDSEM = {"sp": 24, "pool": 24}

    def dma(self, eng, sem, out, in_, reads=(), writes=()):
        reads = list(reads)
        writes = list(writes)
        i = self.dpool_i.get(eng, 0)
        self.dpool_i[eng] = i + 1
        sem = "%s_d%d" % (eng, i % self.NDSEM[eng])
        wl = self._waits(eng, reads, writes)
        prev = self.cnt.get(sem, 0)
        if prev > 0 and self.waited[eng].get(sem, 0) < prev:
            self.waited[eng][sem] = prev
            wl.append((sem, prev))
        self.cnt[sem] = prev + 16
        ev = (sem, self.cnt[sem])
        self.q[eng].append((wl, (lambda e, o=out, i=in_: e.dma_start(out=o, in_=i)), (sem, 16)))
        self._commit(ev, reads, writes)
        return ev

    def final_wait(self, eng, sem):
        self.q[eng].append(([(sem, self.cnt[sem])], None, None))

    def replay(self, name, e, sems):
        for wl, fn, inc in self.q[name]:
            for k, v in wl:
                e.wait_ge(sems[k], v)
            if fn is None:
                continue
            ins = fn(e)
            ins.then_inc(sems[inc[0]], inc[1])


def build_nc(stop_after=None, dbg=None):
    nc = bass.Bass("TRN2", target_bir_lowering=False)
    P = Prog()

    def din(name, shape):
        return nc.dram_tensor(name, list(shape), F32, kind="ExternalInput").ap()

    xr = din("xr", [S, D])
    c_pl = din("c_pl", [128, 16])
    w_ada = din("w_ada", [D, 6 * D])
    bada_pl_d = din("bada_pl", [128, 96])
    gattn_d = din("gattn_pl", [128, 16])
    gffn_d = din("gffn_pl", [128, 16])
    w_in = din("w_in", [D, IN_COLS])
    bgate_d = din("bgate_pl", [128, 32])
    qg_d = din("qg", [1, HD])
    kg_d = din("kg", [1, HD])
    w_a_up = din("w_a_up", [512, D])
    w_b_up = din("w_b_up", [D, D])
    w_out = din("w_out", [D, D])
    w_router = din("w_router", [D, NE])
    ebias_d = din("e_bias", [1, NE])
    w1 = din("w1", [NE, D, 512])
    w3 = din("w3", [NE, D, 512])
    w2 = din("w2", [NE, 512, D])
    ws1 = din("ws1", [D, 512])
    ws3 = din("ws3", [D, 512])
    ws2 = din("ws2", [512, D])
    gfin_d = din("g_final", [1, D])
    ident_d = din("ident", [128, 128])
    mask2_d = din("mask2", [128, 256])
    tabA_d = din("tabA", [S, 64])
    tabB_d = din("tabB", [S, 256])
    kv0_d = din("kv0", [128, 9])
    kv1_d = din("kv1", [128, 12])
    kv2_d = din("kv2", [128, 32])
    out_d = nc.dram_tensor("out", [NT, D], F32, kind="ExternalOutput").ap()

    es = contextlib.ExitStack()
    arena = es.enter_context(nc.sbuf_tensor("arena", [128, ARENA], BF16))
    psum = es.enter_context(nc.psum_tensor("psum", [128, 8, 512], F32))

    def V(off, shape, dt=BF16):
        n = 1
        for s_ in shape[1:]:
            n *= s_
        assert off % 4 == 0
        assert off + n * (4 if dt == F32 else 2) <= ARENA * 2, (off, shape)
        if dt == F32:
            a = arena[:, off // 2: off // 2 + 2 * n].bitcast(F32)
        else:
            a = arena[:, off // 2: off // 2 + n]
        if len(shape) == 3:
            a = a.rearrange("p (a b) -> p a b", b=shape[2])
        elif len(shape) == 4:
            a = a.rearrange("p (a b c) -> p a b c", b=shape[2], c=shape[3])
        if shape[0] < 128:
            a = a[0:shape[0]]
        return a

    def emit(dump=None):
        if dump is not None:
            ap, tiles = dump
            ncol = ap.shape[1]
            P.dma("sp", "do", out_d[0:ap.shape[0], 0:ncol], ap, reads=tiles)
        for sn in list(P.cnt.keys()):
            P.final_wait("sp", sn)
        sem_names = ["pe", "act", "dve"] + ["sp_d%d" % i for i in range(24)] + ["pool_d%d" % i for i in range(24)]
        sems = {n_: es.enter_context(nc.semaphore("s_" + n_)) for n_ in sem_names}
        with nc.Block() as block:
            @block.tensor
            def _(e):
                P.replay("pe", e, sems)

            @block.scalar
            def _(e):
                P.replay("act", e, sems)

            @block.vector
            def _(e):
                P.replay("dve", e, sems)

            @block.gpsimd
            def _(e):
                P.replay("pool", e, sems)

            @block.sync
            def _(e):
                P.replay("sp", e, sems)
        es.close()
        nc._prog_counts = dict(P.cnt)
        nc._prog = P
        return nc

    PB = [T("pb%d" % i) for i in range(8)]

    def pbank(b, dt=F32):
        a = psum[:, b, :]
        return a.bitcast(BF16) if dt == BF16 else a

    def pbanks(b0, nb):
        return psum[:, b0:b0 + nb, :].rearrange("p a b -> p (a b)")

    CB = 5632
    RING_ADDR = [CB, CB + 16384, CB + 32768, 120320]
    XT = 54784
    XN = 71168
    HTT = 79360
    HTO = 87552
    OATA = 120320
    TMP = 128512
    KBF = 136704
    TAB = 138752
    BIG = 140800

    ident_bf = V(0, [128, 128]); ones_bf = V(256, [128, 128])
    ident_f = V(512, [128, 128], F32); mask2 = V(1024, [128, 256], F32)
    modT = V(2048, [128, 96], F32); A1 = V(2432, [128, 16], F32); A2 = V(2496, [128, 16], F32)
    c_act = V(2560, [128, 16], F32); gattn = V(2624, [128, 16], F32); gffn = V(2688, [128, 16], F32)
    bada_pl = V(2752, [128, 96], F32); bgate = V(3136, [128, 32], F32)
    qg_b = V(3264, [128, 128], F32); kg_b = V(3776, [128, 128], F32); ebias_b = V(4288, [128, 64], F32)
    kv0 = V(4544, [128, 9], F32); kv1 = V(4608, [128, 12], F32); kv2 = V(4672, [128, 32], F32)
    ones_f = V(4800, [128, 128], F32)
    scr = V(5312, [128, 64], F32)
    t_const = T("const"); t_modT = T("modT"); t_A1 = T("A1"); t_A2 = T("A2"); t_cact = T("cact")

    P.dma("sp", "dc", c_act, c_pl, writes=[t_cact])
    for dst, src in ((ident_f, ident_d), (mask2, mask2_d), (gattn, gattn_d), (gffn, gffn_d),
                     (bada_pl, bada_pl_d), (bgate, bgate_d), (kv0, kv0_d), (kv1, kv1_d), (kv2, kv2_d),
                     (qg_b, qg_d[0].partition_broadcast(128)), (kg_b, kg_d[0].partition_broadcast(128)),
                     (ebias_b, ebias_d[0].partition_broadcast(128))):
        P.dma("sp", "dc", dst, src, writes=[t_const])
    P.dma("pool", "dw", ident_bf, ident_d, writes=[t_const])
    P.op("dve", lambda e: e.memset(ones_bf, 1.0), writes=[t_const])
    P.op("dve", lambda e: e.memset(ones_f, 1.0), writes=[t_const])
    P.op("act", lambda e: e.activation(out=c_act, in_=c_act, func=AF.Silu), reads=[t_cact], writes=[t_cact])

    if stop_after == 'c0':
        return emit((c_act, [t_cact, t_const]))
    scr_t = [T("scr%d" % i) for i in range(16)]
    scr_i = [0]

    def scratch():
        i = scr_i[0] % 16
        scr_i[0] += 1
        return scr[:, i * 4:i * 4 + 4], scr_t[i]

    ring_t = [T("ring%d" % i) for i in range(4)]
    ring_i = [0]
    ring_n = [3]

    def wslot():
        i = ring_i[0] % ring_n[0]
        ring_i[0] += 1
        return RING_ADDR[i], ring_t[i]

    def wload(src_ap, shape):
        off, t = wslot()
        v = V(off, shape)
        P.dma("pool", "dw", v, src_ap, writes=[t])
        return v, t

    def w_cols(w, c0, n):
        return w[:, c0:c0 + n].rearrange("(k p) n -> p k n", p=128)

    ACC = BIG
    STG = BIG + 24576
    acc = V(ACC, [128, 6144], F32)
    t_acc = T("acc")
    stg_t = [T("stg0"), T("stg1")]
    stg_i = [0]

    def mod_chunk(half, k, pc):
        i = stg_i[0] % 2
        stg_i[0] += 1
        st = V(STG + i * 6144, [128, 1536], F32)
        col0 = half * 6144 + pc * 1536
        P.dma("sp", "dx", st, w_ada[k * 128:(k + 1) * 128, col0:col0 + 1536], writes=[stg_t[i]])
        a = acc[:, pc * 1536:(pc + 1) * 1536]
        if k == 0:
            P.op("dve", lambda e, a=a, st=st: e.tensor_scalar(out=a, in0=st, scalar1=c_act[:, 0:1], scalar2=None,
                                                                op0=ALU.mult),
                 reads=[stg_t[i], t_cact], writes=[t_acc])
        else:
            P.op("dve", lambda e, a=a, st=st, k=k: e.scalar_tensor_tensor(out=a, in0=st, scalar=c_act[:, k:k + 1],
                                                                          in1=a, op0=ALU.mult, op1=ALU.add),
                 reads=[stg_t[i], t_cact], writes=[t_acc])

    def mod_finish(half):
        pm = pbank(7)

        def f(e):
            ins = None
            for j in range(48):
                ins = e.matmul(pm[:, j:j + 1], lhsT=acc[:, j * 128:(j + 1) * 128], rhs=ones_f[:, 0:1],
                               start=True, stop=True)
            return ins
        P.op("pe", f, reads=[t_acc, t_const], writes=[PB[7]])
        P.op("dve", lambda e: e.tensor_tensor(out=modT[:, half * 48:half * 48 + 48], in0=pm[:, 0:48],
                                              in1=bada_pl[:, half * 48:half * 48 + 48], op=ALU.add),
             reads=[PB[7], t_const], writes=[t_modT])

    for k in range(16):
        for pc in range(4):
            mod_chunk(0, k, pc)
    if stop_after == 'c1':
        return emit((acc[:, 0:2048], [t_acc]))
    mod_finish(0)
    if stop_after == 'c2':
        return emit((modT, [t_modT]))
    P.op("dve", lambda e: e.scalar_tensor_tensor(out=A1, in0=modT[:, 16:32], scalar=1.0, in1=gattn,
                                                 op0=ALU.add, op1=ALU.mult),
         reads=[t_modT, t_const], writes=[t_A1])
    B1 = modT[:, 0:16]
    B2 = modT[:, 48:64]
    if stop_after == 'mod1':
        return emit((modT, [t_modT]))
    if stop_after == 'mod1b':
        return emit((A1, [t_A1]))

    h2_chunks = [(k, pc) for k in range(16) for pc in range(4)]
    h2_state = {"i": 0, "done": False}

    def mod_h2_step(n):
        for _ in range(n):
            if h2_state["i"] < len(h2_chunks):
                k, pc = h2_chunks[h2_state["i"]]
                mod_chunk(1, k, pc)
                h2_state["i"] += 1
        if h2_state["i"] == len(h2_chunks) and not h2_state["done"]:
            h2_state["done"] = True
            mod_finish(1)
            P.op("dve", lambda e: e.scalar_tensor_tensor(out=A2, in0=modT[:, 64:80], scalar=1.0, in1=gffn,
                                                         op0=ALU.add, op1=ALU.mult),
                 reads=[t_modT, t_const], writes=[t_A2])

    xt_t = [T("xt0"), T("xt1")]; xn_t = [T("xn0"), T("xn1")]; htt_t = [T("htt0"), T("htt1")]
    prod_i = [0]

    def rms_rstd(src_ap, n, width, src_tiles, junk_ap, junk_tiles):
        ss, tss = scratch()
        P.op("act", lambda e: e.activation(out=junk_ap, in_=src_ap, func=AF.Square, accum_out=ss[0:n, 0:1]),
             reads=src_tiles, writes=junk_tiles + [tss])
        P.op("act", lambda e: e.activation(out=ss[0:n, 1:2], in_=ss[0:n, 0:1], func=AF.Sqrt, scale=1.0 / width,
                                           bias=EPS), reads=[tss], writes=[tss])
        P.op("dve", lambda e: e.reciprocal(out=ss[0:n, 2:3], in_=ss[0:n, 1:2]), reads=[tss], writes=[tss])
        return ss[0:n, 2:3], tss

    htmp_addr = [BIG + 61440]
    t_htmp = T("htmp")

    def transpose_mod(xn, n, xn_tiles, dst, dst_t, Asc, Bsc, t_sc, pb):
        tp = pbanks(pb, 2).bitcast(BF16).rearrange("p (k n) -> p k n", n=128)
        htmp = V(htmp_addr[0], [128, 16, 128])

        def f(e):
            ins = None
            for k in range(16):
                ins = e.transpose(out=tp[:, k, 0:n], in_=xn[:, k * 128:(k + 1) * 128], identity=ident_bf[0:n, 0:n])
            return ins
        P.op("pe", f, reads=xn_tiles + [t_const], writes=[PB[pb], PB[pb + 1]])
        P.op("act", lambda e: e.activation(out=htmp[:, :, 0:n], in_=tp[:, :, 0:n], func=AF.Identity),
             reads=[PB[pb], PB[pb + 1]], writes=[t_htmp])
        P.op("dve", lambda e: e.tensor_tensor(out=htmp[:, :, 0:n], in0=htmp[:, :, 0:n],
                                              in1=Asc.unsqueeze(2).to_broadcast([128, 16, n]), op=ALU.mult),
             reads=[t_sc, t_modT], writes=[t_htmp])
        P.op("dve", lambda e: e.tensor_tensor(out=dst[:, :, 0:n], in0=htmp[:, :, 0:n],
                                              in1=Bsc.unsqueeze(2).to_broadcast([128, 16, n]), op=ALU.add),
             reads=[t_htmp, t_sc, t_modT], writes=[dst_t])

    def produce_a1(rows_ap, n):
        i = prod_i[0] % 2
        prod_i[0] += 1
        xt = V(XT + i * 8192, [128, 2048], F32)[0:n]
        xn = V(XN + i * 4096, [128, 2048])[0:n]
        P.dma("sp", "dx", xt, rows_ap, writes=[xt_t[i]])
        rstd, trs = rms_rstd(xt, n, D, [xt_t[i]], xn, [xn_t[i]])
        P.op("act", lambda e: e.activation(out=xn, in_=xt, func=AF.Identity, scale=rstd),
             reads=[xt_t[i], trs], writes=[xn_t[i]])
        return (xn, n, i)

    def produce_a2(a1, dst, dst_t):
        xn, n, i = a1
        transpose_mod(xn, n, [xn_t[i]], dst, dst_t, A1, B1, t_A1, i * 2)

    def produce(rows_ap, n, dst, dst_t):
        produce_a2(produce_a1(rows_ap, n), dst, dst_t)

    def produce_htt_a2(a1):
        si = a1[2]
        htt = V(HTT + si * 4096, [128, 16, 128])
        produce_a2(a1, htt, htt_t[si])
        return htt, htt_t[si]

    hT_own = V(HTO, [128, 16, 1024])
    hto_t = [T("hto%d" % i) for i in range(8)]
    for j in range(8):
        produce(xr[1024 + j * 128:1024 + (j + 1) * 128, :], 128, hT_own[:, :, j * 128:(j + 1) * 128], hto_t[j])
        mod_h2_step(8)

    if stop_after == 'p1a':
        dd = V(TMP, [128, 1024], F32)
        t_dd = T('dd')
        P.op('dve', lambda e: e.tensor_copy(out=dd.rearrange('p (a b) -> p a b', b=128), in_=hT_own[:, 0:8, 0:128]), reads=hto_t, writes=[t_dd])
        P.op('dve', lambda e: e.tensor_copy(out=dd[:, 0:96], in_=modT), reads=[t_modT], writes=[t_dd])
        return emit((dd, [t_dd]))
    def proj_tm(hT_ap, n, h_tiles, wv, wt, pb):
        po = pbank(pb)

        def f(e):
            ins = None
            for k in range(16):
                ins = e.matmul(po[0:n, :], lhsT=hT_ap[:, k, 0:n], rhs=wv[:, k, :], start=(k == 0), stop=(k == 15))
            return ins
        P.op("pe", f, reads=list(h_tiles) + [wt], writes=[PB[pb]])
        return po

    tmpv = [V(TMP + i * 2048, [128, 512], F32) for i in range(4)]
    tmp_t = [T("tmp%d" % i) for i in range(4)]
    kbf = [V(KBF + i * 1024, [128, 512]) for i in range(2)]
    kbf_t = [T("kbf0"), T("kbf1")]
    kbf_i = [0]
    tab_t = [T("tab0"), T("tab1")]
    tab_i = [0]

    def load_tab(src_ap, n, width):
        i = tab_i[0] % 2
        tab_i[0] += 1
        tv = V(TAB + i * 1024, [128, 256], F32)[0:n, 0:width]
        P.dma("sp", "dx", tv, src_ap, writes=[tab_t[i]])
        return tv, tab_t[i]

    def transpose_heads(kb, n, kt, evac_to_T, pb=6):
        tp = pbank(pb, BF16)[:, 0:512].rearrange("p (h n) -> p h n", n=128)

        def f(e):
            ins = None
            for h in range(4):
                ins = e.transpose(out=tp[:, h, 0:n], in_=kb[:, h * 128:(h + 1) * 128], identity=ident_bf[0:n, 0:n])
            return ins
        P.op("pe", f, reads=[kt, t_const], writes=[PB[pb]])
        evac_to_T(tp, PB[pb])

    def rope_A(po, n, pb, tab, tt, evac_to_T=None):
        i = kbf_i[0] % 2
        kbf_i[0] += 1
        kb = kbf[i][0:n]
        kb3 = kb.rearrange("p (h d) -> p h d", d=128)
        po3 = po[0:n, :].rearrange("p (h d) -> p h d", d=128)
        kf = tmpv[2][0:n, :]
        kf3 = kf.rearrange("p (h d) -> p h d", d=128)
        P.op("act", lambda e: e.activation(out=kf, in_=po[0:n, :], func=AF.Identity), reads=[PB[pb]],
             writes=[tmp_t[2]])
        P.op("dve", lambda e: e.tensor_copy(out=kb, in_=kf), reads=[tmp_t[2]], writes=[kbf_t[i]])
        t1 = tmpv[0][0:n, 0:128].rearrange("p (h d) -> p h d", d=32)
        t2 = tmpv[1][0:n, 0:128].rearrange("p (h d) -> p h d", d=32)
        c1 = tab[:, 0:32].unsqueeze(1).to_broadcast([n, 4, 32])
        s_lo = tab[:, 32:48].unsqueeze(1).to_broadcast([n, 4, 16])
        s_hi = tab[:, 48:64].unsqueeze(1).to_broadcast([n, 4, 16])
        P.op("dve", lambda e: e.tensor_tensor(out=t1, in0=kf3[:, :, 0:32], in1=c1, op=ALU.mult),
             reads=[tmp_t[2], tt], writes=[tmp_t[0]])
        P.op("dve", lambda e: e.tensor_tensor(out=t2[:, :, 0:16], in0=kf3[:, :, 16:32], in1=s_lo, op=ALU.mult),
             reads=[tmp_t[2], tt], writes=[tmp_t[1]])
        P.op("dve", lambda e: e.tensor_tensor(out=t2[:, :, 16:32], in0=kf3[:, :, 0:16], in1=s_hi, op=ALU.mult),
             reads=[tmp_t[2], tt], writes=[tmp_t[1]])
        P.op("dve", lambda e: e.tensor_tensor(out=kb3[:, :, 0:32], in0=t1, in1=t2, op=ALU.add),
             reads=[tmp_t[0], tmp_t[1]], writes=[kbf_t[i]])
        if evac_to_T is None:
            return kb, kbf_t[i]
        transpose_heads(kb, n, kbf_t[i], evac_to_T)

    def rope_B(po, n, pb, tab, tt, g_b, evac_to_T=None, on_dve=False):
        i = kbf_i[0] % 2
        kbf_i[0] += 1
        kb = kbf[i][0:n]
        po3 = po[0:n, :].rearrange("p (h d) -> p h d", d=128)
        ss, tss = scratch()
        junk = tmpv[0][0:n, 0:128]
        for h in range(4):
            P.op("act", lambda e, h=h: e.activation(out=junk, in_=po[0:n, h * 128:(h + 1) * 128], func=AF.Square,
                                                    accum_out=ss[0:n, h:h + 1]),
                 reads=[PB[pb]], writes=[tmp_t[0], tss])
        sq, tsq = scratch()
        P.op("act", lambda e: e.activation(out=sq[0:n, :], in_=ss[0:n, :], func=AF.Sqrt, scale=1.0 / HD, bias=EPS),
             reads=[tss], writes=[tsq])
        P.op("dve", lambda e: e.reciprocal(out=ss[0:n, :], in_=sq[0:n, :]), reads=[tsq], writes=[tss])
        kn = tmpv[1][0:n, :]
        kn3 = kn.rearrange("p (h d) -> p h d", d=128)
        for h in range(4):
            if on_dve:
                P.op("dve", lambda e, h=h: e.tensor_scalar(out=kn[:, h * 128:(h + 1) * 128],
                                                           in0=po[0:n, h * 128:(h + 1) * 128],
                                                           scalar1=ss[0:n, h:h + 1], scalar2=None, op0=ALU.mult),
                     reads=[PB[pb], tss], writes=[tmp_t[1]])
            else:
                P.op("act", lambda e, h=h: e.activation(out=kn[:, h * 128:(h + 1) * 128],
                                                        in_=po[0:n, h * 128:(h + 1) * 128],
                                                        func=AF.Identity, scale=ss[0:n, h:h + 1]),
                     reads=[PB[pb], tss], writes=[tmp_t[1]])
        P.op("dve", lambda e: e.tensor_tensor(out=kn3, in0=kn3, in1=g_b[0:n, :].unsqueeze(1).to_broadcast([n, 4, 128]),
                                              op=ALU.mult), reads=[tmp_t[1], t_const], writes=[tmp_t[1]])
        t1 = tmpv[2][0:n, :].rearrange("p (h d) -> p h d", d=128)
        t2 = tmpv[3][0:n, :].rearrange("p (h a b c) -> p h a b c", a=2, b=2, c=32)
        kn5 = kn.rearrange("p (h a b c) -> p h a b c", a=2, b=2, c=32)
        cs2 = tab[:, 128:256].rearrange("p (a b c) -> p a b c", a=2, b=2, c=32)
        P.op("dve", lambda e: e.tensor_tensor(out=t1, in0=kn3, in1=tab[:, 0:128].unsqueeze(1).to_broadcast([n, 4, 128]),
                                              op=ALU.mult), reads=[tmp_t[1], tt], writes=[tmp_t[2]])
        for bsel in range(2):
            P.op("dve", lambda e, bsel=bsel: e.tensor_tensor(
                out=t2[:, :, :, bsel, :], in0=kn5[:, :, :, 1 - bsel, :],
                in1=cs2[:, :, bsel, :].unsqueeze(1).to_broadcast([n, 4, 2, 32]), op=ALU.mult),
                reads=[tmp_t[1], tt], writes=[tmp_t[3]])
        P.op("dve", lambda e: e.tensor_tensor(out=kb, in0=tmpv[2][0:n, :], in1=tmpv[3][0:n, :], op=ALU.add),
             reads=[tmp_t[2], tmp_t[3]], writes=[kbf_t[i]])
        if evac_to_T is None:
            return kb, kbf_t[i]
        transpose_heads(kb, n, kbf_t[i], evac_to_T)

    def run_pipeline(tiles):
        n_ = len(tiles)

        def a1(i_):
            if 0 <= i_ < n_ and tiles[i_][0] is not None:
                tiles[i_][0][0]()

        def a2(i_):
            if 0 <= i_ < n_ and tiles[i_][0] is not None:
                tiles[i_][0][1]()
        a1(0)
        a1(1)
        a2(0)
        for i_ in range(n_):
            a1(i_ + 2)
            a2(i_ + 1)
            tiles[i_][1]()
            if i_ > 0:
                tiles[i_ - 1][2]()
        if n_:
            tiles[n_ - 1][2]()

    ACCO = BIG
    ACCZ = BIG + 16384
    QAT = BIG + 32768
    KAT = QAT + 8192
    VA = KAT + 8192
    PTA = VA + 8192
    EXA = PTA + 2048
    accO = V(ACCO, [128, 4, 1024], F32); accZ = V(ACCZ, [128, 4, 1024], F32)
    t_accO = T("accO"); t_accZ = T("accZ")
    qaT = V(QAT, [128, 4, 1024]); t_qaT = [T("qaT%d" % i) for i in range(8)]
    pta_t = [T("pta%d" % i) for i in range(3)]; exa_t = [T("exa%d" % i) for i in range(3)]
    alias([t_accO, t_accZ] + t_qaT + pta_t + exa_t, [t_acc] + stg_t)

    units = []
    for u in range(4):
        units.append((2, [(cl, 0, 1) for cl in range(4 * u, 4 * u + 4)]))
    for u in range(2):
        units.append((1, [(cl, 0, 2) for cl in (2 * u, 2 * u + 1)]))
    units.append((0, [(0, 0, 4)]))
    units.append((0, [(0, 4, 8)]))

    prev_kv_tiles = []
    cur_g = None
    for (gi, items) in units:
        r = A_DIL[gi]
        nq = 1024 // r
        qb = min(nq, 128)
        nkt = nq // qb + 1
        kvalid = (kv0, kv1, kv2)[gi]

        def ktsz(m, gi=gi):
            return 64 if (gi == 2 and m == 1) else 128
        if cur_g != gi:
            cur_g = gi
            wq, wq_t = wload(w_cols(w_in, gi * 1536, 512), [128, 16, 512])
            wk, wk_t = wload(w_cols(w_in, gi * 1536 + 512, 512), [128, 16, 512])
            wv, wv_t = wload(w_cols(w_in, gi * 1536 + 1024, 512), [128, 16, 512])
            alias(t_qaT, prev_kv_tiles)
            qtiles = []
            for j in range(8):
                st_ = {}

                def evq(tp, pt, j=j, r=r, nq=nq):
                    w = 128 // r
                    qtmp = V(TMP + 3 * 2048, [128, 512]); tq_ = tmp_t[3]
                    P.op("act", lambda e: e.activation(out=qtmp, in_=tp.rearrange("p h n -> p (h n)"), func=AF.Identity),
                         reads=[pt], writes=[tq_])
                    dstv = qaT.rearrange("p h (c u) -> p h c u", u=nq)[:, :, :, j * w:(j + 1) * w]
                    srcv = qtmp.rearrange("p (h u c) -> p h c u", h=4, c=r)
                    P.op("dve", lambda e: e.tensor_copy(out=dstv, in_=srcv), reads=[tq_], writes=[t_qaT[j]])

                def Bq(j=j, st_=st_, wq=wq, wq_t=wq_t):
                    pb_ = 4 + (j % 2)
                    po = proj_tm(hT_own[:, :, j * 128:(j + 1) * 128], 128, [hto_t[j]], wq, wq_t, pb_)
                    tab, tt = load_tab(tabA_d[1024 + j * 128:1024 + (j + 1) * 128, :], 128, 64)
                    st_["kb"] = rope_A(po, 128, pb_, tab, tt)

                def Cq(st_=st_, evq=evq):
                    kb, kbt = st_["kb"]
                    transpose_heads(kb, 128, kbt, evq)
                qtiles.append((None, Bq, Cq))
            run_pipeline(qtiles)
        if stop_after == 'q':
            dd = V(ACCO, [128, 2048], F32); t_dd = T('dd')
            P.op('dve', lambda e: e.tensor_copy(out=dd, in_=qaT[:, 0:2, :]) if False else e.tensor_copy(out=dd.rearrange('p (a b) -> p a b', b=1024), in_=qaT[:, 0:2, :]), reads=t_qaT, writes=[t_dd])
            return emit((dd, [t_dd]))
        nit = len(items)
        kstride = max((b_hi - b_lo + 1) for (_, b_lo, b_hi) in items) * 128
        kaT = V(KAT, [128, 4, nit * kstride])
        t_ka = [T("ka%d" % i) for i in range(nit)]
        t_va = [T("va%d" % i) for i in range(nit)]
        alias(t_ka + t_va, prev_kv_tiles)
        K0 = nq - 64 if r == 1 else (1024 // r) - 64
        xr_c = xr.rearrange("(u c) d -> c u d", c=r)
        tabA_c = tabA_d.rearrange("(u c) d -> c u d", c=r)
        vslot = {}
        sl = 0
        ktiles = []
        tix = 0
        for ii, (cl, b_lo, b_hi) in enumerate(items):
            for m in range(b_lo, b_hi + 1):
                n = ktsz(m)
                u0 = K0 + 128 * m
                kpos = ii * kstride + (m - b_lo) * 128
                vv = V(VA + sl * 1024, [128, 512])[0:n]
                sl += 1
                vslot[(ii, m)] = vv
                st_ = {}

                def Ak1(st_=st_, cl=cl, u0=u0, n=n):
                    st_["a1"] = produce_a1(xr_c[cl, u0:u0 + n, :], n)

                def Ak2(st_=st_):
                    st_["h"] = produce_htt_a2(st_["a1"])

                def Bk(st_=st_, cl=cl, u0=u0, n=n, tix=tix, vv=vv, ii=ii, wk=wk, wk_t=wk_t, wv=wv, wv_t=wv_t, t_va=t_va):
                    htt, htt_tile = st_["h"]
                    pb_ = 4 + (tix % 2)
                    pk = proj_tm(htt, n, [htt_tile], wk, wk_t, pb_)
                    pv = proj_tm(htt, n, [htt_tile], wv, wv_t, 7)
                    tab, tt = load_tab(tabA_c[cl, u0:u0 + n, :], n, 64)
                    st_["kb"] = rope_A(pk, n, pb_, tab, tt)
                    P.op("act", lambda e: e.activation(out=vv, in_=pv[0:n, :], func=AF.Identity),
                         reads=[PB[7]], writes=[t_va[ii]])

                def Ck(st_=st_, kpos=kpos, n=n, ii=ii, kaT=kaT, t_ka=t_ka):
                    kb, kbt = st_["kb"]

                    def evk(tp, pt):
                        for h in range(4):
                            P.op("dve", lambda e, h=h: e.tensor_copy(out=kaT[:, h, kpos:kpos + n], in_=tp[:, h, 0:n]),
                                 reads=[pt], writes=[t_ka[ii]])
                    transpose_heads(kb, n, kbt, evk)
                ktiles.append(((Ak1, Ak2), Bk, Ck))
                tix += 1
        run_pipeline(ktiles)
        if stop_after == 'kv':
            dd = V(ACCO, [128, 2048], F32); t_dd = T('dd')
            P.op('dve', lambda e: e.tensor_copy(out=dd[:, 0:1024], in_=kaT[:, 0, 0:1024]), reads=t_ka, writes=[t_dd])
            P.op('dve', lambda e: e.tensor_copy(out=dd[:, 1024:2048], in_=V(VA, [128, 1024])), reads=t_va, writes=[t_dd])
            return emit((dd, [t_dd]))
        steps = []
        for ii, (cl, b_lo, b_hi) in enumerate(items):
            for h in range(4):
                for m in range(b_lo, b_hi + 1):
                    steps.append((ii, cl, b_lo, b_hi, h, m))

        def S_step(k, steps=steps, kaT=kaT, t_ka=t_ka, kvalid=kvalid, qb=qb, nq=nq, nkt=nkt, kstride=kstride, ktsz=ktsz):
            ii, cl, b_lo, b_hi, h, m = steps[k]
            n = ktsz(m)
            blocks = [bq for bq in (m - 1, m) if b_lo <= bq < b_hi]
            c_lo = blocks[0] * qb
            ncols = len(blocks) * qb
            bi = k % 3
            sbk = (0, 1, 6)[bi]
            sb = pbank(sbk)
            kpos = ii * kstride + (m - b_lo) * 128
            qcol0 = cl * nq
            P.op("pe", lambda e: e.matmul(sb[0:n, 0:ncols], lhsT=kaT[:, h, kpos:kpos + n],
                                          rhs=qaT[:, h, qcol0 + c_lo:qcol0 + c_lo + ncols], start=True, stop=True),
                 reads=[t_ka[ii]] + t_qaT, writes=[PB[sbk]])
            ex = V(EXA + bi * 512, [128, 256])[0:n, 0:ncols]
            pt_ = V(PTA + bi * 512, [128, 256])[0:n, 0:ncols]
            P.op("act", lambda e: e.activation(out=ex, in_=sb[0:n, 0:ncols], func=AF.Exp, scale=SCALE),
                 reads=[PB[sbk]], writes=[exa_t[bi]])
            if len(blocks) == 2:
                mk = mask2[0:n, 0:256]
            elif blocks[0] == m:
                mk = mask2[0:n, 128:128 + qb]
            else:
                mk = mask2[0:n, 0:qb]
            kvc = cl * nkt + m
            P.op("dve", lambda e: e.scalar_tensor_tensor(out=pt_, in0=ex, scalar=kvalid[0:n, kvc:kvc + 1], in1=mk,
                                                         op0=ALU.mult, op1=ALU.mult),
                 reads=[exa_t[bi], t_const], writes=[pta_t[bi]])
            return (pt_, blocks, c_lo, n, bi)

        def PV_step(k, info, steps=steps, vslot=vslot, t_va=t_va, qb=qb, r=r, gi=gi):
            ii, cl, b_lo, b_hi, h, m = steps[k]
            pt_, blocks, c_lo, n, bi = info
            ob = 2 + (h % 2)
            zb = 4 + (h % 2)
            vv = vslot[(ii, m)]

            def g(e):
                ins = None
                for bq in blocks:
                    col = bq * qb - c_lo
                    first = (m == bq)
                    last = (m == bq + 1)
                    po_ = pbank(ob)[:, (bq - b_lo) * qb:(bq - b_lo + 1) * qb]
                    pz_ = pbank(zb)[:, (bq - b_lo) * qb:(bq - b_lo + 1) * qb]
                    e.matmul(po_, lhsT=vv[:, h * 128:(h + 1) * 128], rhs=pt_[:, col:col + qb],
                             start=first, stop=last, skip_group_check=True)
                    ins = e.matmul(pz_, lhsT=ones_bf[0:n, :], rhs=pt_[:, col:col + qb], start=first,
                                   stop=last, skip_group_check=True)
                return ins
            P.op("pe", g, reads=[pta_t[bi], t_va[ii], t_const], writes=[PB[ob], PB[zb]])
            if m == b_hi:
                nqc = (b_hi - b_lo) * qb
                po_all = pbank(ob)[:, 0:nqc]
                pz_all = pbank(zb)[:, 0:nqc]
                dO = accO[:, h, :].rearrange("p (u c) -> p c u", c=r)[:, cl, b_lo * qb:b_hi * qb]
                dZ = accZ[:, h, :].rearrange("p (u c) -> p c u", c=r)[:, cl, b_lo * qb:b_hi * qb]
                eo = tmpv[0][:, 0:nqc]
                ez = tmpv[1][:, 0:nqc]
                P.op("act", lambda e: e.activation(out=eo, in_=po_all, func=AF.Identity),
                     reads=[PB[ob]], writes=[tmp_t[0]])
                P.op("act", lambda e: e.activation(out=ez, in_=pz_all, func=AF.Identity),
                     reads=[PB[zb]], writes=[tmp_t[1]])
                if gi == 2:
                    P.op("dve", lambda e: e.tensor_copy(out=dO, in_=eo), reads=[tmp_t[0]], writes=[t_accO])
                    P.op("dve", lambda e: e.tensor_copy(out=dZ, in_=ez), reads=[tmp_t[1]], writes=[t_accZ])
                else:
                    P.op("dve", lambda e: e.tensor_tensor(out=dO, in0=dO, in1=eo, op=ALU.add),
                         reads=[tmp_t[0]], writes=[t_accO])
                    P.op("dve", lambda e: e.tensor_tensor(out=dZ, in0=dZ, in1=ez, op=ALU.add),
                         reads=[tmp_t[1]], writes=[t_accZ])
        infos = {0: S_step(0)}
        if len(steps) > 1:
            infos[1] = S_step(1)
        for k in range(len(steps)):
            if k + 2 < len(steps):
                infos[k + 2] = S_step(k + 2)
            PV_step(k, infos[k])
        prev_kv_tiles = t_ka + t_va + pta_t + exa_t
        if stop_after == 'u0':
            return emit((accO[:, 0, :], [t_accO, t_accZ]))

    oaT = V(OATA, [128, 4, 1024]); t_oaT = T("oaT")
    accOf = V(ACCO, [128, 4096], F32); accZf = V(ACCZ, [128, 4096], F32)
    P.op("dve", lambda e: e.reciprocal(out=accZf, in_=accZf), reads=[t_accZ], writes=[t_accZ])
    P.op("dve", lambda e: e.tensor_tensor(out=V(OATA, [128, 4096]), in0=accOf, in1=accZf, op=ALU.mult),
         reads=[t_accO, t_accZ], writes=[t_oaT])

    if stop_after == 'p1b':
        dd = V(BIG, [128, 2048], F32)
        t_dd = T('dd')
        alias([t_dd], [t_accO, t_accZ])
        P.op('dve', lambda e: e.tensor_copy(out=dd, in_=V(OATA, [128, 4096])[:, 0:2048]), reads=[t_oaT], writes=[t_dd])
        return emit((dd, [t_dd]))
    ring_n[0] = 2
    ring_i[0] = 0
    kbT = V(BIG, [128, 4, 4096]); vB = V(BIG + 32768, [128, 32, 512])
    t_kb = [T("kb%d" % j) for j in range(32)]
    t_vb = [T("vb%d" % j) for j in range(32)]
    alias(t_kb + t_vb, [t_accO, t_accZ] + t_qaT + prev_kv_tiles)
    wkB, wkB_t = wload(w_cols(w_in, 4608 + 2048, 512), [128, 16, 512])
    wvB, wvB_t = wload(w_cols(w_in, 4608 + 2560, 512), [128, 16, 512])
    kvtiles = []
    for j in range(32):
        st_ = {}
        own = 8 <= j < 16

        def Ab1(st_=st_, j=j):
            st_["a1"] = produce_a1(xr[j * 128:(j + 1) * 128, :], 128)

        def Ab2(st_=st_):
            st_["h"] = produce_htt_a2(st_["a1"])

        def Bb(st_=st_, j=j, own=own):
            if own:
                h_ap = hT_own[:, :, (j - 8) * 128:(j - 7) * 128]
                h_t = [hto_t[j - 8]]
            else:
                h_ap, ht_ = st_["h"]
                h_t = [ht_]
            pb_ = 4 + (j % 2)
            pk = proj_tm(h_ap, 128, h_t, wkB, wkB_t, pb_)
            pv = proj_tm(h_ap, 128, h_t, wvB, wvB_t, 7)
            tab, tt = load_tab(tabB_d[j * 128:(j + 1) * 128, :], 128, 256)
            st_["kb"] = rope_B(pk, 128, pb_, tab, tt, kg_b)
            P.op("act", lambda e: e.activation(out=vB[:, j, :], in_=pv, func=AF.Identity),
                 reads=[PB[7]], writes=[t_vb[j]])

        def Cb(st_=st_, j=j):
            kb, kbt = st_["kb"]

            def evk(tp, pt):
                for h in range(4):
                    P.op("dve", lambda e, h=h: e.tensor_copy(out=kbT[:, h, j * 128:(j + 1) * 128], in_=tp[:, h, :]),
                         reads=[pt], writes=[t_kb[j]])
            transpose_heads(kb, 128, kbt, evk)
        kvtiles.append((None if own else (Ab1, Ab2), Bb, Cb))
    htmp_addr[0] = RING_ADDR[2]
    alias([t_htmp], [ring_t[2]] + prev_kv_tiles + [t_accO, t_accZ] + t_qaT)
    run_pipeline(kvtiles)
    alias([ring_t[2]], [t_htmp])

    ring_n[0] = 1
    ring_i[0] = 0
    QB_ADDR = [RING_ADDR[2], RING_ADDR[1]]
    PTB = RING_ADDR[2] + 8192
    EPB = PTB + 4096
    qbTs = [V(a_, [128, 4, 1024]) for a_ in QB_ADDR]
    t_qbs = [[T("qb%d_%d" % (s_, i)) for i in range(8)] for s_ in range(2)]
    ptb = [V(PTB + i * 1024, [128, 512]) for i in range(4)]
    ptb_t = [T("ptb%d" % i) for i in range(4)]
    epb = V(EPB, [128, 512], F32); t_epb = T("epb")
    alias(t_qbs[0] + ptb_t + [t_epb], [ring_t[2]])
    alias(t_qbs[1], [ring_t[1]])
    t_qb = t_qbs[0] + t_qbs[1]
    obT = V(XT, [128, 16, 1024])
    t_ob = [T("ob%d" % i) for i in range(32)]
    alias(t_ob, xt_t + xn_t + htt_t)

    def q_tiles(kvh):
        wqB, wqB_t = wload(w_cols(w_in, 4608 + kvh * 512, 512), [128, 16, 512])
        qbT = qbTs[kvh % 2]
        tq = t_qbs[kvh % 2]
        tl = []
        for jt in range(8):
            st_ = {}

            def Bq(st_=st_, jt=jt):
                pb_ = 6
                pq = proj_tm(hT_own[:, :, jt * 128:(jt + 1) * 128], 128, [hto_t[jt]], wqB, wqB_t, pb_)
                tab, tt = load_tab(tabB_d[1024 + jt * 128:1024 + (jt + 1) * 128, :], 128, 256)
                st_["kb"] = rope_B(pq, 128, pb_, tab, tt, qg_b, on_dve=True)

            def Cq(st_=st_, jt=jt):
                kb, kbt = st_["kb"]

                def evq(tp, pt):
                    for h in range(4):
                        P.op("dve", lambda e, h=h: e.tensor_copy(out=qbT[:, h, jt * 128:(jt + 1) * 128], in_=tp[:, h, :]),
                             reads=[pt], writes=[tq[jt]])
                transpose_heads(kb, 128, kbt, evq, pb=6)
            tl.append((None, Bq, Cq))
        return tl

    def attn(kvh, inter):
        qbT = qbTs[kvh % 2]
        tq = t_qbs[kvh % 2]
        steps = [(hh, half, t) for hh in range(4) for half in range(2) for t in range(32)]

        def issue_S(si):
            hh, half, t = steps[si]
            sbk = (0, 1, 7)[si % 3]
            pi = si % 4
            P.op("pe", lambda e: e.matmul(pbank(sbk), lhsT=kbT[:, kvh, t * 128:(t + 1) * 128],
                                          rhs=qbT[:, hh, half * 512:(half + 1) * 512], start=True, stop=True),
                 reads=[t_kb[t]] + tq[half * 4:half * 4 + 4], writes=[PB[sbk]])
            P.op("act", lambda e: e.activation(out=ptb[pi], in_=pbank(sbk), func=AF.Exp, scale=SCALE),
                 reads=[PB[sbk]], writes=[ptb_t[pi]])

        def issue_PV(si):
            hh, half, t = steps[si]
            pi = si % 4
            sel = (hh * 2 + half) % 2
            ob, zb = 2 + sel, 4 + sel

            def g(e):
                e.matmul(pbank(ob), lhsT=vB[:, t, kvh * 128:(kvh + 1) * 128], rhs=ptb[pi], start=(t == 0),
                         stop=(t == 31))
                return e.matmul(pbank(zb), lhsT=ones_bf, rhs=ptb[pi], start=(t == 0), stop=(t == 31))
            P.op("pe", g, reads=[ptb_t[pi], t_vb[t], t_const], writes=[PB[ob], PB[zb]])
            if t == 31:
                head = kvh * 4 + hh
                P.op("dve", lambda e: e.reciprocal(out=epb, in_=pbank(zb)), reads=[PB[zb]], writes=[t_epb])
                P.op("dve", lambda e: e.tensor_tensor(out=obT[:, head, half * 512:(half + 1) * 512], in0=pbank(ob),
                                                      in1=epb, op=ALU.mult),
                     reads=[PB[ob], t_epb], writes=[t_ob[head * 2 + half]])
        inter_ops = []
        if inter:
            n_ = len(inter)
            for i_ in range(n_):
                inter_ops.append(inter[i_][1])
                if i_ > 0:
                    inter_ops.append(inter[i_ - 1][2])
            inter_ops.append(inter[n_ - 1][2])
        issue_S(0)
        issue_S(1)
        for si in range(len(steps)):
            if si + 2 < len(steps):
                issue_S(si + 2)
            issue_PV(si)
            if si % 14 == 7 and inter_ops:
                inter_ops.pop(0)()
        while inter_ops:
            inter_ops.pop(0)()

    run_pipeline(q_tiles(0))
    for kvh in range(4):
        attn(kvh, q_tiles(kvh + 1) if kvh + 1 < 4 else None)
    if stop_after == 'p2':
        dd = V(BIG, [128, 2048], F32)
        t_dd = T('dd')
        alias([t_dd], t_kb + t_vb)
        P.op('dve', lambda e: e.tensor_copy(out=dd.rearrange('p (a b) -> p a b', b=1024), in_=obT[:, 0:2, :]), reads=t_ob, writes=[t_dd])
        return emit((dd, [t_dd]))
    ring_n[0] = 3
    ring_i[0] = 0
    alias([ring_t[2]], t_qbs[0] + ptb_t + [t_epb])
    alias([ring_t[1]], t_qbs[1])
    mT = V(BIG, [128, 16, 1024]); t_mT = [T("mT%d" % i) for i in range(32)]
    alias(t_mT, t_kb + t_vb)
    G3 = BIG + 32768
    g3v = [V(G3 + i * 2048, [128, 512], F32) for i in range(8)]
    g3t = [T("g3_%d" % i) for i in range(8)]
    alias(g3t, t_kb + t_vb)
    for dc in range(16):
        off, wt = wslot()
        wga = V(off, [128, 16, 128]); wgb = V(off + 4096, [128, 16, 128])
        wbu = V(off + 8192, [128, 16, 128]); wau = V(off + 12288, [128, 4, 128])
        P.dma("pool", "dw", wga, w_cols(w_in, 7680 + dc * 128, 128), writes=[wt])
        P.dma("pool", "dw", wgb, w_cols(w_in, 7680 + 2048 + dc * 128, 128), writes=[wt])
        P.dma("pool", "dw", wbu, w_cols(w_b_up, dc * 128, 128), writes=[wt])
        P.dma("pool", "dw", wau, w_cols(w_a_up, dc * 128, 128), writes=[wt])
        for half in range(2):
            hs = slice(half * 512, (half + 1) * 512)
            bga, bgb, bya, byb = half, 2 + half, 4 + half, 6 + half

            def fg(e, wsrc, bank, hs=hs):
                ins = None
                for k in range(16):
                    ins = e.matmul(pbank(bank), lhsT=wsrc[:, k, :], rhs=hT_own[:, k, hs], start=(k == 0), stop=(k == 15))
                return ins
            P.op("pe", lambda e, wga=wga, bga=bga, fg=fg: fg(e, wga, bga), reads=[wt] + hto_t[half * 4:half * 4 + 4],
                 writes=[PB[bga]])
            P.op("pe", lambda e, wgb=wgb, bgb=bgb, fg=fg: fg(e, wgb, bgb), reads=[wt] + hto_t[half * 4:half * 4 + 4],
                 writes=[PB[bgb]])

            def fya(e, wau=wau, bya=bya, hs=hs):
                ins = None
                for h in range(4):
                    ins = e.matmul(pbank(bya), lhsT=wau[:, h, :], rhs=oaT[:, h, hs], start=(h == 0), stop=(h == 3))
                return ins
            P.op("pe", fya, reads=[wt, t_oaT], writes=[PB[bya]])

            def fyb(e, wbu=wbu, byb=byb, hs=hs):
                ins = None
                for h in range(16):
                    ins = e.matmul(pbank(byb), lhsT=wbu[:, h, :], rhs=obT[:, h, hs], start=(h == 0), stop=(h == 15))
                return ins
            P.op("pe", fyb, reads=[wt] + [t_ob[h * 2 + half] for h in range(16)], writes=[PB[byb]])
            sga, sgb, t1, t2 = [g3v[half * 4 + i] for i in range(4)]
            tga, tgb, tt1, tt2 = [g3t[half * 4 + i] for i in range(4)]
            P.op("act", lambda e, sga=sga, bga=bga, dc=dc: e.activation(out=sga, in_=pbank(bga), func=AF.Sigmoid,
                                                                        bias=bgate[:, dc:dc + 1]),
                 reads=[PB[bga], t_const], writes=[tga])
            P.op("act", lambda e, sgb=sgb, bgb=bgb, dc=dc: e.activation(out=sgb, in_=pbank(bgb), func=AF.Sigmoid,
                                                                        bias=bgate[:, 16 + dc:17 + dc]),
                 reads=[PB[bgb], t_const], writes=[tgb])
            P.op("dve", lambda e, t1=t1, sga=sga, bya=bya: e.tensor_tensor(out=t1, in0=sga, in1=pbank(bya), op=ALU.mult),
                 reads=[tga, PB[bya]], writes=[tt1])
            P.op("dve", lambda e, t2=t2, sgb=sgb, byb=byb: e.tensor_tensor(out=t2, in0=sgb, in1=pbank(byb), op=ALU.mult),
                 reads=[tgb, PB[byb]], writes=[tt2])
            P.op("dve", lambda e, t1=t1, t2=t2, dc=dc, hs=hs: e.tensor_tensor(out=mT[:, dc, hs], in0=t1, in1=t2,
                                                                              op=ALU.add),
                 reads=[tt1, tt2], writes=[t_mT[dc * 2 + half]])

    def bcast_pl(src_pl, src_t, dst, dst_t, dg_off, dg_alias):
        dg = V(dg_off, [128, 16, 128], F32)
        t_dg = T("dg")
        alias([t_dg], dg_alias)
        for c in range(16):
            P.op("dve", lambda e, c=c: e.tensor_scalar(out=dg[:, c, :], in0=ident_f, scalar1=src_pl[:, c:c + 1],
                                                       scalar2=None, op0=ALU.mult),
                 reads=[t_const, src_t], writes=[t_dg])
        pbv = pbanks(0, 4)

        def f(e):
            ins = None
            for c in range(16):
                ins = e.matmul(pbv[:, c * 128:(c + 1) * 128], lhsT=ones_f, rhs=dg[:, c, :], start=True, stop=True)
            return ins
        P.op("pe", f, reads=[t_dg, t_const], writes=PB[0:4])
        P.op("act", lambda e: e.activation(out=dst, in_=pbv, func=AF.Identity), reads=PB[0:4], writes=[dst_t])

    ring_n[0] = 4
    alias([ring_t[3]], [t_oaT] + tmp_t + kbf_t + tab_t)
    x1 = V(XT, [128, 8, 2048], F32)
    t_x1 = [[T("x1_%d_%d" % (j, c)) for c in range(4)] for j in range(8)]
    old = t_ob + hto_t + xt_t + xn_t + htt_t
    for j in range(8):
        alias(t_x1[j], old)
    gt1b = V(G3, [128, 2048], F32); t_gt1b = T("gt1b")
    alias([t_gt1b], g3t)
    bcast_pl(modT[:, 32:48], t_modT, gt1b, t_gt1b, G3 + 8192, g3t)
    for j in range(8):
        P.dma("sp", "dx", x1[:, j, :], xr[1024 + j * 128:1024 + (j + 1) * 128, :], writes=t_x1[j])
    ytmp = [V(G3 + 16384 + i * 2048, [128, 512], F32) for i in range(2)]
    ytmp_t = [T("ytmp0"), T("ytmp1")]
    alias(ytmp_t, g3t)
    blk = 0
    for cb in range(4):
        wo, wo_t = wload(w_cols(w_out, cb * 512, 512), [128, 16, 512])
        for j in range(8):
            bank = blk % 4
            yt, ytt = ytmp[blk % 2], ytmp_t[blk % 2]
            blk += 1

            def f(e, bank=bank, j=j, wo=wo):
                ins = None
                for k in range(16):
                    ins = e.matmul(pbank(bank), lhsT=mT[:, k, j * 128:(j + 1) * 128], rhs=wo[:, k, :], start=(k == 0),
                                   stop=(k == 15))
                return ins
            P.op("pe", f, reads=[wo_t] + t_mT[(j // 4)::2], writes=[PB[bank]])
            cs = slice(cb * 512, (cb + 1) * 512)
            P.op("dve", lambda e, yt=yt, bank=bank, cs=cs: e.tensor_tensor(out=yt, in0=pbank(bank), in1=gt1b[:, cs],
                                                                           op=ALU.mult),
                 reads=[PB[bank], t_gt1b], writes=[ytt])
            P.op("dve", lambda e, yt=yt, j=j, cs=cs: e.tensor_tensor(out=x1[:, j, cs], in0=x1[:, j, cs], in1=yt,
                                                                     op=ALU.add),
                 reads=[ytt], writes=[t_x1[j][cb]])

    if stop_after == 'p3':
        return emit((x1[:, 0, :], t_x1[0]))
    h2T = V(BIG, [128, 16, 1024]); t_h2 = [T("h2_%d" % j) for j in range(8)]
    alias(t_h2, t_mT)
    GT2 = G3
    HID = G3 + 4096
    STMP = HID + 8192
    GATE = STMP + 4096
    RT = GATE + 2048
    XN2 = RT + 2048
    WR = XN2 + 8192
    GT2F = HID
    assert WR + 2048 <= ARENA * 2
    gt2b = V(GT2, [128, 2048]); t_gt2b = T("gt2b")
    hidT = V(HID, [128, 4, 1024]); t_hid = [[T("hid%d%d" % (fc, hf)) for hf in range(2)] for fc in range(4)]
    stmp = [V(STMP + i * 2048, [128, 512], F32) for i in range(2)]; stmp_t = [T("st0"), T("st1")]
    gate = V(GATE, [128, 8, 64], F32); t_gate = [T("gate%d" % j) for j in range(8)]
    rt = [V(RT + i * 256, [128, 64], F32) for i in range(8)]; rt_t = [T("rt%d" % i) for i in range(8)]
    xn2 = [V(XN2 + i * 4096, [128, 2048]) for i in range(2)]; xn2_t = [T("xn2_0"), T("xn2_1")]
    wr = V(WR, [128, 16, 64]); t_wr = T("wr")
    gt2f = V(GT2F, [128, 2048], F32); t_gt2f = T("gt2f")
    p4 = [t_gt2b] + [t for l in t_hid for t in l] + stmp_t + t_gate + rt_t + xn2_t + [t_wr, t_gt2f]
    alias(p4, g3t + [t_gt1b] + ytmp_t + t_kb + t_vb)
    P.dma("pool", "dw", wr, w_router.rearrange("(k p) n -> p k n", p=128), writes=[t_wr])
    bcast_pl(modT[:, 80:96], t_modT, gt2f, t_gt2f, XN2, xn2_t)
    P.op("dve", lambda e: e.tensor_copy(out=gt2b, in_=gt2f), reads=[t_gt2f], writes=[t_gt2b])
    alias([t for l in t_hid for t in l], [t_gt2f])
    htmp_addr[0] = HID + 4096
    alias([t_htmp], [t_gt2f])
    p4st = [dict() for _ in range(8)]

    def P4a(j):
        i = j % 2
        rstd, trs = rms_rstd(x1[:, j, :], 128, D, t_x1[j], xn2[i], [xn2_t[i]])
        P.op("dve", lambda e, i=i, j=j, rstd=rstd: e.tensor_scalar(out=xn2[i], in0=x1[:, j, :], scalar1=rstd,
                                                                   scalar2=None, op0=ALU.mult),
             reads=t_x1[j] + [trs], writes=[xn2_t[i]])

    def P4b(j):
        i = j % 2
        transpose_mod(xn2[i], 128, [xn2_t[i]], h2T[:, :, j * 128:(j + 1) * 128], t_h2[j], A2, B2, t_A2, i * 2)

    def P4c(j):
        i = j % 2
        pl = pbank(4 + i)[:, 0:64]

        def fr(e, j=j, pl=pl):
            ins = None
            for k in range(16):
                ins = e.matmul(pl, lhsT=h2T[:, k, j * 128:(j + 1) * 128], rhs=wr[:, k, :], start=(k == 0),
                               stop=(k == 15))
            return ins
        P.op("pe", fr, reads=[t_h2[j], t_wr], writes=[PB[4 + i]])
        routing(j, i, pl)

    def routing(j, i, pl):
            sc_, sel, eq, selm, em = rt[0], rt[1], rt[2], rt[3], rt[4]
            sm = rt[5]
            sel3 = sel.rearrange("p (g e) -> p g e", e=8)
            eq3 = eq.rearrange("p (g e) -> p g e", e=8)
            selm3 = selm.rearrange("p (g e) -> p g e", e=8)
            R = rt_t
            P.op("act", lambda e, pl=pl: e.activation(out=sc_, in_=pl, func=AF.Sigmoid), reads=[PB[4 + i]], writes=[R[0]])
            P.op("dve", lambda e: e.tensor_tensor(out=sel, in0=sc_, in1=ebias_b, op=ALU.add), reads=[R[0], t_const],
                 writes=[R[1]])
            P.op("dve", lambda e: e.tensor_reduce(out=sm[:, 0:8], in_=sel3, axis=AX.X, op=ALU.max), reads=[R[1]],
                 writes=[R[5]])
            P.op("dve", lambda e: e.tensor_tensor(out=eq3, in0=sel3, in1=sm[:, 0:8].unsqueeze(2).to_broadcast([128, 8, 8]),
                                                  op=ALU.is_ge), reads=[R[1], R[5]], writes=[R[2]])
            P.op("dve", lambda e: e.scalar_tensor_tensor(out=selm, in0=eq, scalar=-1e30, in1=sel, op0=ALU.mult,
                                                         op1=ALU.add), reads=[R[2], R[1]], writes=[R[3]])
            P.op("dve", lambda e: e.tensor_reduce(out=sm[:, 8:16], in_=selm3, axis=AX.X, op=ALU.max), reads=[R[3]],
                 writes=[R[5]])
            P.op("dve", lambda e: e.tensor_tensor(out=sm[:, 8:16], in0=sm[:, 8:16], in1=sm[:, 0:8], op=ALU.add),
                 reads=[R[5]], writes=[R[5]])
            P.op("dve", lambda e: e.max(out=sm[:, 16:24], in_=sm[:, 8:16]), reads=[R[5]], writes=[R[5]])
            P.op("dve", lambda e: e.tensor_scalar(out=sm[:, 24:32], in0=sm[:, 8:16], scalar1=sm[:, 19:20], scalar2=None,
                                                  op0=ALU.is_ge), reads=[R[5]], writes=[R[5]])
            P.op("dve", lambda e: e.tensor_scalar(out=sm[:, 24:32], in0=sm[:, 24:32], scalar1=1e30, scalar2=-1e30,
                                                  op0=ALU.mult, op1=ALU.add), reads=[R[5]], writes=[R[5]])
            P.op("dve", lambda e: e.tensor_tensor(out=selm3, in0=sel3,
                                                  in1=sm[:, 24:32].unsqueeze(2).to_broadcast([128, 8, 8]), op=ALU.add),
                 reads=[R[1], R[5]], writes=[R[3]])
            P.op("dve", lambda e: e.max(out=sm[:, 32:40], in_=selm), reads=[R[3]], writes=[R[5]])
            P.op("dve", lambda e: e.tensor_scalar(out=em, in0=selm, scalar1=sm[:, 39:40], scalar2=None, op0=ALU.is_ge),
                 reads=[R[3], R[5]], writes=[R[4]])
            P.op("dve", lambda e: e.tensor_tensor(out=em, in0=em, in1=sc_, op=ALU.mult), reads=[R[4], R[0]],
                 writes=[R[4]])
            P.op("dve", lambda e: e.tensor_reduce(out=sm[:, 40:41], in_=em, axis=AX.X, op=ALU.add), reads=[R[4]],
                 writes=[R[5]])
            P.op("dve", lambda e: e.reciprocal(out=sm[:, 41:42], in_=sm[:, 40:41]), reads=[R[5]], writes=[R[5]])
            P.op("dve", lambda e, j=j: e.tensor_scalar(out=gate[:, j, :], in0=em, scalar1=sm[:, 41:42], scalar2=2.5,
                                                       op0=ALU.mult, op1=ALU.mult), reads=[R[4], R[5]],
                 writes=[t_gate[j]])

    P4a(0)
    P4a(1)
    P4b(0)
    for j in range(8):
        if j + 2 < 8:
            P4a(j + 2)
        if j + 1 < 8:
            P4b(j + 1)
        P4c(j)
    alias([t for l in t_hid for t in l], [t_htmp])
    if stop_after == 'p4r':
        dd = V(XN2, [128, 2048], F32)
        t_dd = T('dd')
        alias([t_dd], xn2_t)
        P.op('dve', lambda e: e.tensor_copy(out=dd[:, 0:512], in_=gate.rearrange('p a b -> p (a b)')), reads=t_gate, writes=[t_dd])
        P.op('dve', lambda e: e.tensor_copy(out=dd[:, 512:1024].rearrange('p (a b) -> p a b', b=128), in_=h2T[:, 0:4, 0:128]), reads=t_h2, writes=[t_dd])
        return emit((dd[:, 0:1024], [t_dd]))
    yblk = [0]
    for ex_i in range(NE + (1 if stop_after != 'noshared' else 0)):
        if ex_i < NE:
            s1, s3, s2 = w1[ex_i], w3[ex_i], w2[ex_i]
        else:
            s1, s3, s2 = ws1, ws3, ws2
        w1v, w1t = wload(s1.rearrange("(k p) n -> p k n", p=128), [128, 16, 512])
        w3v, w3t = wload(s3.rearrange("(k p) n -> p k n", p=128), [128, 16, 512])
        w2v, w2t = wload(s2.rearrange("(f p) n -> p f n", p=128), [128, 4, 2048])
        P.op("dve", lambda e, w2v=w2v: e.tensor_tensor(out=w2v, in0=w2v,
                                                       in1=gt2b.unsqueeze(1).to_broadcast([128, 4, 2048]), op=ALU.mult),
             reads=[w2t, t_gt2b], writes=[w2t])
        u = 0
        for fc in range(4):
            for half in range(2):
                ba, bu = 2 * (u % 2), 2 * (u % 2) + 1
                hs = slice(half * 512, (half + 1) * 512)

                def fup(e, wsrc, bank, fc=fc, hs=hs):
                    ins = None
                    for k in range(16):
                        ins = e.matmul(pbank(bank), lhsT=wsrc[:, k, fc * 128:(fc + 1) * 128], rhs=h2T[:, k, hs],
                                       start=(k == 0), stop=(k == 15))
                    return ins
                P.op("pe", lambda e, fup=fup, w1v=w1v, ba=ba: fup(e, w1v, ba), reads=[w1t] + t_h2[half * 4:half * 4 + 4],
                     writes=[PB[ba]])
                P.op("pe", lambda e, fup=fup, w3v=w3v, bu=bu: fup(e, w3v, bu), reads=[w3t] + t_h2[half * 4:half * 4 + 4],
                     writes=[PB[bu]])
                st, stt = stmp[u % 2], stmp_t[u % 2]
                P.op("act", lambda e, st=st, ba=ba: e.activation(out=st, in_=pbank(ba), func=AF.Silu),
                     reads=[PB[ba]], writes=[stt])
                P.op("dve", lambda e, st=st, bu=bu, fc=fc, hs=hs: e.tensor_tensor(out=hidT[:, fc, hs], in0=st,
                                                                                  in1=pbank(bu), op=ALU.mult),
                     reads=[stt, PB[bu]], writes=[t_hid[fc][half]])
                u += 1
        for j in range(8):
            for cb in range(4):
                bank = 4 + (yblk[0] % 4)
                yblk[0] += 1
                cs = slice(cb * 512, (cb + 1) * 512)

                def fdn(e, bank=bank, j=j, cs=cs, w2v=w2v):
                    ins = None
                    for fc in range(4):
                        ins = e.matmul(pbank(bank), lhsT=hidT[:, fc, j * 128:(j + 1) * 128], rhs=w2v[:, fc, cs],
                                       start=(fc == 0), stop=(fc == 3))
                    return ins
                P.op("pe", fdn, reads=[w2t] + [t_hid[fc][j // 4] for fc in range(4)], writes=[PB[bank]])
                if ex_i < NE:
                    P.op("dve", lambda e, bank=bank, j=j, cs=cs, ex_i=ex_i: e.scalar_tensor_tensor(
                        out=x1[:, j, cs], in0=pbank(bank), scalar=gate[:, j, ex_i:ex_i + 1], in1=x1[:, j, cs],
                        op0=ALU.mult, op1=ALU.add), reads=[PB[bank], t_gate[j]], writes=[t_x1[j][cb]])
                else:
                    P.op("dve", lambda e, bank=bank, j=j, cs=cs: e.tensor_tensor(
                        out=x1[:, j, cs], in0=x1[:, j, cs], in1=pbank(bank), op=ALU.add),
                        reads=[PB[bank]], writes=[t_x1[j][cb]])

    gfin = gt2f
    alias([t_gt2f], [t for l in t_hid for t in l])
    P.dma("sp", "dc", gfin, gfin_d[0].partition_broadcast(128), writes=[t_gt2f])
    for j in range(8):
        i = 0
        rstd, trs = rms_rstd(x1[:, j, :], 128, D, t_x1[j], xn2[i], [xn2_t[i]])
        P.op("dve", lambda e, j=j, rstd=rstd: e.scalar_tensor_tensor(out=x1[:, j, :], in0=x1[:, j, :], scalar=rstd,
                                                                     in1=gfin, op0=ALU.mult, op1=ALU.mult),
             reads=[trs, t_gt2f], writes=t_x1[j])
        P.dma("sp", "do", out_d[j * 128:(j + 1) * 128, :], x1[:, j, :], reads=t_x1[j])
    return emit()


def _host_inputs(inputs):
    x = np.asarray(inputs["x"], np.float32)
    c = np.asarray(inputs["c"], np.float32)

    def pl(v):
        v = np.asarray(v, np.float32).reshape(-1, 128)
        return np.ascontiguousarray(v.T)

    shared = {
        "w_ada": np.ascontiguousarray(np.asarray(inputs["w_ada"], np.float32)[0]),
        "bada_pl": pl(np.asarray(inputs["b_ada"])[0]),
        "gattn_pl": pl(np.asarray(inputs["g_attn"])[0]),
        "gffn_pl": pl(np.asarray(inputs["g_ffn"])[0]),
        "w_in": np.ascontiguousarray(np.asarray(inputs["w_in"], np.float32)[0]),
        "bgate_pl": pl(np.asarray(inputs["b_gate"])[0]),
        "qg": np.asarray(inputs["q_norm_g"], np.float32).reshape(1, HD),
        "kg": np.asarray(inputs["k_norm_g"], np.float32).reshape(1, HD),
        "w_a_up": np.ascontiguousarray(np.asarray(inputs["w_a_up"], np.float32)[0]),
        "w_b_up": np.ascontiguousarray(np.asarray(inputs["w_b_up"], np.float32)[0]),
        "w_out": np.ascontiguousarray(np.asarray(inputs["w_out"], np.float32)[0]),
        "w_router": np.ascontiguousarray(np.asarray(inputs["w_router"], np.float32)[0]),
        "e_bias": np.asarray(inputs["e_bias"], np.float32).reshape(1, NE),
        "w1": np.ascontiguousarray(np.asarray(inputs["w1"], np.float32)[0]),
        "w3": np.ascontiguousarray(np.asarray(inputs["w3"], np.float32)[0]),
        "w2": np.ascontiguousarray(np.asarray(inputs["w2"], np.float32)[0]),
        "ws1": np.ascontiguousarray(np.asarray(inputs["ws1"], np.float32)[0]),
        "ws3": np.ascontiguousarray(np.asarray(inputs["ws3"], np.float32)[0]),
        "ws2": np.ascontiguousarray(np.asarray(inputs["ws2"], np.float32)[0]),
        "g_final": np.asarray(inputs["g_final"], np.float32).reshape(1, D),
        "ident": np.eye(128, dtype=np.float32),
    }
    ii = np.arange(128)[:, None]
    cc = np.arange(128)[None, :]
    shared["mask2"] = np.concatenate([(cc >= ii), (cc <= ii)], axis=1).astype(np.float32)
    pos = np.arange(S, dtype=np.float32)
    invA = np.power(np.float32(500000.0), -np.arange(0, 32, 2, dtype=np.float32) / 32).astype(np.float32)
    angA = pos[:, None] * invA[None, :]
    cA, sA = np.cos(angA).astype(np.float32), np.sin(angA).astype(np.float32)
    tabA = np.concatenate([cA, cA, -sA, sA], axis=1).astype(np.float32)
    invB = np.power(np.float32(10000.0), -np.arange(0, 64, 2, dtype=np.float32) / 64).astype(np.float32)
    row = (np.arange(S) // 64).astype(np.float32)
    col = (np.arange(S) % 64).astype(np.float32)
    ar, ac = row[:, None] * invB[None, :], col[:, None] * invB[None, :]
    cr, sr, cc_, sc_ = np.cos(ar), np.sin(ar), np.cos(ac), np.sin(ac)
    tabB = np.concatenate([cr, cr, cc_, cc_, -sr, sr, -sc_, sc_], axis=1).astype(np.float32)
    in_maps = []
    for core in range(8):
        b, q = core // 4, core % 4
        s0 = q * 1024
        shift = s0 - 1024
        tok = (np.arange(S) + shift) % S
        m = dict(shared)
        m["xr"] = np.ascontiguousarray(x[b][tok])
        m["c_pl"] = pl(c[b])
        m["tabA"] = np.ascontiguousarray(tabA[tok])
        m["tabB"] = np.ascontiguousarray(tabB[tok])
        kvs = []
        for gi, r in enumerate(A_DIL):
            nq = 1024 // r
            qb = min(nq, 128)
            nkt = nq // qb + 1
            K0 = 1024 // r - 64
            kv = np.zeros((128, r * nkt), np.float32)
            for cl in range(r):
                for mm in range(nkt):
                    j = cl + r * (K0 + 128 * mm + np.arange(128))
                    t = j + shift
                    kv[:, cl * nkt + mm] = ((t >= 0) & (t < S) & (j < S)).astype(np.float32)
            kvs.append(kv)
        m["kv0"], m["kv1"], m["kv2"] = kvs
        in_maps.append(m)
    return in_maps


_NC_CACHE = {}


def kernel(**inputs):
    in_maps = _host_inputs(inputs)
    if "nc" not in _NC_CACHE:
        _NC_CACHE["nc"] = build_nc()
    nc = _NC_CACHE["nc"]
    res = run_bass_kernel_spmd(nc, in_maps, core_ids=list(range(8)))
    out = np.zeros((2, S, D), np.float32)
    for core in range(8):
        b, q = core // 4, core % 4
        out[b, q * 1024:(q + 1) * 1024] = res.results[core]["out"]
    return out
```
